# Optimizing a Trainium2 kernel written in Bass

```python
import math
import jax, jax.numpy as jnp
from jax import lax
import numpy as np

D_MODEL = 1024
BATCH = 1
SEQ = 16384
DEPTH = 1

DN_HEADS = 4
DN_HEAD_DIM = 128
DN_WIDTH = DN_HEADS * DN_HEAD_DIM
CONV_WIDTH = 4
DN_CHUNK = 64
DA_HEADS = 4
DA_HEAD_DIM = 64
DA_WIDTH = DA_HEADS * 2 * DA_HEAD_DIM
Q_BLOCK = 128
ROPE_THETA = 10000.0
N_EXPERTS = 32
TOP_K = 4
D_EXPERT = D_MODEL
SWIGLU_LIMIT = 7.0
SWIGLU_ALPHA = 1.702
MOE_BLOCK = 256
N_MODS = 6
IN_SIZES = (DN_WIDTH, DN_WIDTH, DN_WIDTH, DN_WIDTH, DN_HEADS, DN_HEADS,
            DA_WIDTH, DA_WIDTH, DA_WIDTH, D_MODEL, D_MODEL)
IN_OFFSETS = tuple(sum(IN_SIZES[:i + 1]) for i in range(len(IN_SIZES) - 1))
IN_COLS = sum(IN_SIZES)

kernel_name = 'hybrid_deltanet_diffattn_moe_deepnorm_adaln'


def _deepnorm_alpha():
    return (2.0 * DEPTH) ** 0.25


def _deepnorm_beta():
    return (8.0 * DEPTH) ** -0.25


def _layer_norm(x, g, b, eps=1e-5):
    xf = x.astype(jnp.float32)
    mu = jnp.mean(xf, -1, keepdims=True)
    var = jnp.mean(jnp.square(xf - mu), -1, keepdims=True)
    return ((xf - mu) * lax.rsqrt(var + eps) * g + b).astype(x.dtype)


def _rms_norm(x, w, eps=1e-5):
    xf = x.astype(jnp.float32)
    return (xf * lax.rsqrt(jnp.mean(xf * xf, -1, keepdims=True) + eps) * w).astype(x.dtype)


def _l2_normalize(x, eps=1e-6):
    xf = x.astype(jnp.float32)
    return (xf * lax.rsqrt(jnp.sum(xf * xf, -1, keepdims=True) + eps)).astype(x.dtype)


def _causal_depthwise_conv(x, w):
    k, ch = w.shape
    return lax.conv_general_dilated(x, w[:, None, :].astype(x.dtype), window_strides=(1,),
                                    padding=[(k - 1, 0)], dimension_numbers=('NWC', 'WIO', 'NWC'),
                                    feature_group_count=ch)


def _rope(x, positions):
    d = x.shape[-1]
    half = d // 2
    inv_freq = ROPE_THETA ** (-jnp.arange(half, dtype=jnp.float32) / half)
    ang = positions.astype(jnp.float32)[..., None] * inv_freq
    ang = ang.reshape(ang.shape[:2] + (1,) * (x.ndim - 3) + (half,))
    cos, sin = jnp.cos(ang), jnp.sin(ang)
    xf = x.astype(jnp.float32)
    x1, x2 = xf[..., :half], xf[..., half:]
    return jnp.concatenate([x1 * cos - x2 * sin, x2 * cos + x1 * sin], -1).astype(x.dtype)


def _gated_delta_rule(q, k, v, g, beta):
    out_dtype = v.dtype
    b, s, h, dk = q.shape
    dv = v.shape[-1]
    c = DN_CHUNK
    n = s // c

    def to_chunks(t):
        t = t.astype(jnp.float32).reshape((b, n, c, h) + t.shape[3:])
        return jnp.moveaxis(t, 3, 1)

    q, k, v, g, beta = (to_chunks(t) for t in (q, k, v, g, beta))
    cum_g = jnp.cumsum(g, axis=-1)
    idx = jnp.arange(c)
    strict = idx[:, None] > idx[None, :]
    incl = idx[:, None] >= idx[None, :]
    decay = jnp.exp(jnp.where(incl, cum_g[..., :, None] - cum_g[..., None, :], -jnp.inf))
    kk = jnp.einsum('bhnid,bhnjd->bhnij', k, k)
    a_mat = jnp.where(strict, beta[..., None] * decay * kk, 0.0)
    lower = jnp.eye(c, dtype=jnp.float32) + a_mat
    rhs = jnp.concatenate([beta[..., None] * v, (beta * jnp.exp(cum_g))[..., None] * k], -1)
    sol = lax.linalg.triangular_solve(lower, rhs, left_side=True, lower=True)
    u_base, w_mat = sol[..., :dv], sol[..., dv:]
    qk = jnp.einsum('bhnid,bhnjd->bhnij', q, k) * decay
    q_dec = q * jnp.exp(cum_g)[..., None]
    k_dec = k * jnp.exp(cum_g[..., -1:] - cum_g)[..., None]
    chunk_decay = jnp.exp(cum_g[..., -1])

    def step(state, inp):
        u_b, w_c, qk_c, qd_c, kd_c, cd_c = inp
        u = u_b - jnp.einsum('bhcd,bhde->bhce', w_c, state)
        o = jnp.einsum('bhcd,bhde->bhce', qd_c, state) + jnp.einsum('bhij,bhje->bhie', qk_c, u)
        state = state * cd_c[..., None, None] + jnp.einsum('bhcd,bhce->bhde', kd_c, u)
        return state, o

    xs = tuple(jnp.moveaxis(t, 2, 0) for t in (u_base, w_mat, qk, q_dec, k_dec, chunk_decay))
    state0 = jnp.zeros((b, h, dk, dv), jnp.float32)
    _, o = lax.scan(step, state0, xs)
    o = jnp.transpose(o, (1, 0, 3, 2, 4)).reshape(b, s, h, dv)
    return o.astype(out_dtype)


def _diff_attention(q, k, v, lam):
    b, s, h, _, d = q.shape
    nblk = s // Q_BLOCK
    scale = d ** -0.5
    kpos = jnp.arange(s)

    def one_block(i):
        start = i * Q_BLOCK
        qb = lax.dynamic_slice_in_dim(q, start, Q_BLOCK, axis=1)
        sc = jnp.einsum('bqhmd,bkhmd->bhmqk', qb, k, preferred_element_type=jnp.float32) * scale
        qpos = start + jnp.arange(Q_BLOCK)
        mask = kpos[None, :] <= qpos[:, None]
        p = jax.nn.softmax(jnp.where(mask, sc, -jnp.inf), axis=-1)
        a = p[:, :, 0] - lam * p[:, :, 1]
        return jnp.einsum('bhqk,bkhe->bqhe', a.astype(v.dtype), v)

    o = lax.map(one_block, jnp.arange(nblk))
    return jnp.moveaxis(o, 0, 1).reshape(b, s, h, 2 * d)


def _clamped_swiglu(gu):
    x_glu, x_lin = gu[..., ::2], gu[..., 1::2]
    x_glu = jnp.minimum(x_glu, SWIGLU_LIMIT)
    x_lin = jnp.clip(x_lin, -SWIGLU_LIMIT, SWIGLU_LIMIT)
    return x_glu * jax.nn.sigmoid(SWIGLU_ALPHA * x_glu) * (x_lin + 1.0)


def _moe(h, w_router, b_router, w_gate_up, b_gate_up, w_down, b_down):
    b, s, d = h.shape
    m = b * s
    hf = h.reshape(m, d)
    logits = (hf @ w_router + b_router).astype(jnp.float32)
    top_val, top_idx = lax.top_k(logits, TOP_K)
    top_w = jax.nn.softmax(top_val, axis=-1)
    n_assign = m * TOP_K
    expert_flat = top_idx.reshape(-1).astype(jnp.int32)
    token_flat = jnp.arange(n_assign, dtype=jnp.int32) // TOP_K
    weight_flat = top_w.reshape(-1)
    order = jnp.argsort(expert_flat)
    e_sorted = expert_flat[order]
    counts = jnp.bincount(expert_flat, length=N_EXPERTS).astype(jnp.int32)
    group_start = jnp.cumsum(counts) - counts
    padded = (counts + MOE_BLOCK - 1) // MOE_BLOCK * MOE_BLOCK
    padded_end = jnp.cumsum(padded)
    padded_start = padded_end - padded
    rank = jnp.arange(n_assign, dtype=jnp.int32) - group_start[e_sorted]
    dest = padded_start[e_sorted] + rank
    n_blocks = -(-n_assign // MOE_BLOCK) + N_EXPERTS
    n_rows = n_blocks * MOE_BLOCK
    row_token = jnp.full((n_rows,), m, jnp.int32).at[dest].set(token_flat[order])
    row_weight = jnp.zeros((n_rows,), jnp.float32).at[dest].set(weight_flat[order])
    block_start = jnp.arange(n_blocks, dtype=jnp.int32) * MOE_BLOCK
    block_expert = jnp.minimum(jnp.searchsorted(padded_end, block_start, side='right'), N_EXPERTS - 1)
    h_pad = jnp.concatenate([hf, jnp.zeros((1, d), hf.dtype)], 0)

    def expert_block(args):
        rows, e = args
        xb = h_pad[rows]
        gu = xb @ w_gate_up[e] + b_gate_up[e]
        return _clamped_swiglu(gu) @ w_down[e] + b_down[e]

    out = lax.map(expert_block, (row_token.reshape(n_blocks, MOE_BLOCK), block_expert))
    out = out.reshape(n_rows, d) * row_weight.astype(out.dtype)[:, None]
    y = jnp.zeros((m + 1, d), out.dtype).at[row_token].add(out)[:m]
    return y.reshape(b, s, d)


def _mixer(u, positions, layer, w_in, conv_w, dn_a_log, dn_dt_bias, dn_norm_w, w_dn_proj,
           lambda_q1, lambda_k1, lambda_q2, lambda_k2, da_norm_w, w_da_proj, w_o):
    b, s, _ = u.shape
    proj = u @ w_in
    qkv_end = IN_OFFSETS[2]
    qkv = jax.nn.silu(_causal_depthwise_conv(proj[..., :qkv_end], conv_w))
    dq, dk, dv = jnp.split(qkv, 3, axis=-1)
    z, b_logit, a_in, aq, ak, av, g_dn, g_da = jnp.split(
        proj[..., qkv_end:], [o - qkv_end for o in IN_OFFSETS[3:]], axis=-1)

    dq = _l2_normalize(dq.reshape(b, s, DN_HEADS, DN_HEAD_DIM)) * (DN_HEAD_DIM ** -0.5)
    dk = _l2_normalize(dk.reshape(b, s, DN_HEADS, DN_HEAD_DIM))
    dv = dv.reshape(b, s, DN_HEADS, DN_HEAD_DIM)
    beta = jax.nn.sigmoid(b_logit.astype(jnp.float32))
    g = -jnp.exp(dn_a_log.astype(jnp.float32)) * jax.nn.softplus(
        a_in.astype(jnp.float32) + dn_dt_bias.astype(jnp.float32))
    o_dn = _gated_delta_rule(dq, dk, dv, g, beta)
    o_dn = _rms_norm(o_dn, dn_norm_w) * jax.nn.silu(z.reshape(b, s, DN_HEADS, DN_HEAD_DIM))
    y_dn = o_dn.reshape(b, s, DN_WIDTH) @ w_dn_proj

    lam_init = 0.8 - 0.6 * math.exp(-0.3 * layer)
    lam = (jnp.exp(jnp.sum(lambda_q1.astype(jnp.float32) * lambda_k1.astype(jnp.float32)))
           - jnp.exp(jnp.sum(lambda_q2.astype(jnp.float32) * lambda_k2.astype(jnp.float32))) + lam_init)
    aq = _rope(aq.reshape(b, s, DA_HEADS, 2, DA_HEAD_DIM), positions)
    ak = _rope(ak.reshape(b, s, DA_HEADS, 2, DA_HEAD_DIM), positions)
    av = av.reshape(b, s, DA_HEADS, 2 * DA_HEAD_DIM)
    o_da = _diff_attention(aq, ak, av, lam)
    o_da = _rms_norm(o_da, da_norm_w) * (1.0 - lam_init)
    y_da = o_da.reshape(b, s, DA_WIDTH) @ w_da_proj

    merged = jax.nn.sigmoid(g_dn) * y_dn + jax.nn.sigmoid(g_da) * y_da
    return merged @ w_o


def setup_inputs(seed: int = 0) -> dict:
    key = jax.random.key(seed)
    keys = jax.random.split(key, 48)
    counter = [0]

    def nxt():
        k = keys[counter[0]]
        counter[0] += 1
        return k

    def nrm(shape, scale):
        return jax.random.normal(nxt(), shape, jnp.float32) * scale

    L, D = DEPTH, D_MODEL
    beta = _deepnorm_beta()
    x = nrm((BATCH, SEQ, D), 1.0)
    c = nrm((BATCH, D), 1.0)
    positions = jnp.broadcast_to(jnp.arange(SEQ, dtype=jnp.int32), (BATCH, SEQ))
    w_ada = nrm((L, D, N_MODS * D), D ** -0.5)
    b_ada = nrm((L, N_MODS * D), 0.02)
    in_gains = (1.0, 1.0, beta, 1.0, 1.0, 1.0, 1.0, 1.0, beta, 1.0, 1.0)
    w_in = jnp.concatenate([nrm((L, D, n), gn * D ** -0.5) for n, gn in zip(IN_SIZES, in_gains)], -1)
    conv_w = nrm((L, CONV_WIDTH, 3 * DN_WIDTH), CONV_WIDTH ** -0.5)
    dn_a_log = jnp.log(jax.random.uniform(nxt(), (L, DN_HEADS), jnp.float32, 1.0, 16.0))
    dt = jnp.exp(jax.random.uniform(nxt(), (L, DN_HEADS), jnp.float32)
                 * (math.log(0.1) - math.log(0.001)) + math.log(0.001))
    dn_dt_bias = dt + jnp.log(-jnp.expm1(-dt))
    dn_norm_w = 1.0 + nrm((L, DN_HEAD_DIM), 0.02)
    w_dn_proj = nrm((L, DN_WIDTH, D), DN_WIDTH ** -0.5)
    lambda_q1 = nrm((L, DA_HEAD_DIM), 0.1)
    lambda_k1 = nrm((L, DA_HEAD_DIM), 0.1)
    lambda_q2 = nrm((L, DA_HEAD_DIM), 0.1)
    lambda_k2 = nrm((L, DA_HEAD_DIM), 0.1)
    da_norm_w = 1.0 + nrm((L, 2 * DA_HEAD_DIM), 0.02)
    w_da_proj = nrm((L, DA_WIDTH, D), DA_WIDTH ** -0.5)
    w_o = nrm((L, D, D), beta * D ** -0.5)
    ln1_g = 1.0 + nrm((L, D), 0.02)
    ln1_b = nrm((L, D), 0.02)
    w_router = nrm((L, D, N_EXPERTS), D ** -0.5)
    b_router = nrm((L, N_EXPERTS), 0.01)
    w_gate_up = nrm((L, N_EXPERTS, D, 2 * D_EXPERT), beta * D ** -0.5)
    b_gate_up = nrm((L, N_EXPERTS, 2 * D_EXPERT), 0.02)
    w_down = nrm((L, N_EXPERTS, D_EXPERT, D), beta * D_EXPERT ** -0.5)
    b_down = nrm((L, N_EXPERTS, D), 0.02)
    ln2_g = 1.0 + nrm((L, D), 0.02)
    ln2_b = nrm((L, D), 0.02)
    return {'x': x, 'c': c, 'positions': positions, 'w_ada': w_ada, 'b_ada': b_ada, 'w_in': w_in,
            'conv_w': conv_w, 'dn_a_log': dn_a_log, 'dn_dt_bias': dn_dt_bias, 'dn_norm_w': dn_norm_w,
            'w_dn_proj': w_dn_proj, 'lambda_q1': lambda_q1, 'lambda_k1': lambda_k1,
            'lambda_q2': lambda_q2, 'lambda_k2': lambda_k2, 'da_norm_w': da_norm_w,
            'w_da_proj': w_da_proj, 'w_o': w_o, 'ln1_g': ln1_g, 'ln1_b': ln1_b,
            'w_router': w_router, 'b_router': b_router, 'w_gate_up': w_gate_up,
            'b_gate_up': b_gate_up, 'w_down': w_down, 'b_down': b_down, 'ln2_g': ln2_g, 'ln2_b': ln2_b}


def reference(x, c, positions, w_ada, b_ada, w_in, conv_w, dn_a_log, dn_dt_bias, dn_norm_w,
              w_dn_proj, lambda_q1, lambda_k1, lambda_q2, lambda_k2, da_norm_w, w_da_proj, w_o,
              ln1_g, ln1_b, w_router, b_router, w_gate_up, b_gate_up, w_down, b_down, ln2_g, ln2_b):
    alpha = _deepnorm_alpha()
    silu_c = jax.nn.silu(c)
    for l in range(DEPTH):
        mods = (silu_c @ w_ada[l] + b_ada[l])[:, None, :]
        shift_m, scale_m, gate_m, shift_f, scale_f, gate_f = jnp.split(mods, N_MODS, axis=-1)
        u = x * (1.0 + scale_m) + shift_m
        mix = _mixer(u, positions, l, w_in[l], conv_w[l], dn_a_log[l], dn_dt_bias[l], dn_norm_w[l],
                     w_dn_proj[l], lambda_q1[l], lambda_k1[l], lambda_q2[l], lambda_k2[l],
                     da_norm_w[l], w_da_proj[l], w_o[l])
        x = _layer_norm(alpha * x + gate_m * mix, ln1_g[l], ln1_b[l])
        u = x * (1.0 + scale_f) + shift_f
        ffn = _moe(u, w_router[l], b_router[l], w_gate_up[l], b_gate_up[l], w_down[l], b_down[l])
        x = _layer_norm(alpha * x + gate_f * ffn, ln2_g[l], ln2_b[l])
    return x
```

```python
import numpy as np
import ml_dtypes
import concourse.bass as bass
import concourse.mybir as mybir
from concourse.bass_utils import run_bass_kernel_spmd

F32 = mybir.dt.float32
BF16 = mybir.dt.bfloat16
I32 = mybir.dt.int32
ALU = mybir.AluOpType
AF = mybir.ActivationFunctionType
AX = mybir.AxisListType

ENGS = ("pe", "act", "dve", "pool", "sp")
NCORES = 8
D = 1024
TL = 256
EPS_L2 = 1e-6
EPS_N = 1e-5
MAGIC = 12582912.0
TWO_PI = 6.283185307179586
C1 = 6.28125
C2 = TWO_PI - C1


class Prog:
    def __init__(self, nc, same_engine_sync=True):
        self.nc = nc
        self.ops = {e: [] for e in ENGS}
        self.cnt = {e: 0 for e in ENGS}
        self.esem = {}
        self.res = {}
        self.dsem = {}
        self.same = same_engine_sync
        self._stack = []
        self.known = {e: {} for e in ENGS}

    def _cm(self, cm):
        v = cm.__enter__()
        self._stack.append(cm)
        return v

    def sem(self, name):
        return self._cm(self.nc.semaphore(name))

    def sb(self, name, shape, dt):
        return self._cm(self.nc.sbuf_tensor("s_" + name, list(shape), dt))

    def ps(self, name, shape, dt):
        return self._cm(self.nc.psum_tensor(name, list(shape), dt))

    def close(self):
        while self._stack:
            self._stack.pop().__exit__(None, None, None)

    def _deps(self, eng, r, w, skip_same):
        toks = []
        for k in r:
            st = self.res.get(k)
            if st and st[0] is not None:
                toks.append(st[0])
        for k in w:
            st = self.res.get(k)
            if st:
                if st[0] is not None:
                    toks.append(st[0])
                toks.extend(st[1])
        best = {}
        for (s, v, e) in toks:
            if e == eng and (skip_same or not self.same):
                continue
            key = id(s)
            if key not in best or best[key][1] < v:
                best[key] = (s, v, e)
        out = []
        kn = self.known[eng]
        for key, (s, v, e) in best.items():
            if kn.get(key, -1) >= v:
                continue
            kn[key] = v
            out.append((s, v))
        return out

    def _commit(self, tok, r, w):
        for k in r:
            st = self.res.setdefault(k, [None, []])
            st[1].append(tok)
        for k in w:
            self.res[k] = [tok, []]

    def op(self, eng, fn, r=(), w=(), skip_same=False):
        pr = [k for k in r if isinstance(k, tuple) and k[0] == "pb"]
        if pr:
            r = [k for k in r if k not in pr]
            w = list(w) + [k for k in pr if k not in w]
        waits = self._deps(eng, r, w, skip_same)
        if eng not in self.esem:
            self.esem[eng] = self.sem("es_" + eng)
        self.cnt[eng] += 1
        tok = (self.esem[eng], self.cnt[eng], eng)
        self.ops[eng].append((waits, fn, (self.esem[eng], 1)))
        self._commit(tok, r, w)
        return tok

    def dma(self, eng, semkey, fn, r=(), w=()):
        waits = self._deps(eng, r, w, False)
        if semkey not in self.dsem:
            self.dsem[semkey] = [self.sem("ds_%d" % len(self.dsem)), 0]
        d = self.dsem[semkey]
        d[1] += 16
        tok = (d[0], d[1], "dma")
        self.ops[eng].append((waits, fn, (d[0], 16)))
        self._commit(tok, r, w)
        return tok

    def final_wait(self, eng="sp"):
        toks = {}
        for k, st in self.res.items():
            for t in ([st[0]] if st[0] is not None else []) + st[1]:
                key = id(t[0])
                if key not in toks or toks[key][1] < t[1]:
                    toks[key] = t
        self.ops[eng].append(([(s, v) for (s, v, e) in toks.values()], None, None))

    def run(self):
        nc = self.nc
        with nc.Block() as block:
            def replay(name):
                def f(e):
                    for waits, fn, inc in self.ops[name]:
                        for (s, v) in waits:
                            e.wait_ge(s, v)
                        if fn is not None:
                            fn(e).then_inc(inc[0], inc[1])
                return f
            block.tensor(replay("pe"))
            block.scalar(replay("act"))
            block.vector(replay("dve"))
            block.gpsimd(replay("pool"))
            block.sync(replay("sp"))


class Em:
    def __init__(self, P):
        self.P = P

    def mm(self, out, lhsT, rhs, start=True, stop=True, r=(), w=()):
        return self.P.op("pe", lambda e: e.matmul(out, lhsT=lhsT, rhs=rhs, start=start, stop=stop), r=r, w=w, skip_same=True)

    def tr(self, out, in_, ident, r=(), w=()):
        return self.P.op("pe", lambda e: e.transpose(out, in_, ident), r=r, w=w, skip_same=True)

    def act(self, out, in_, func, bias=0.0, scale=1.0, r=(), w=(), accum_out=None):
        if accum_out is None:
            return self.P.op("act", lambda e: e.activation(out=out, in_=in_, func=func, bias=bias, scale=scale), r=r, w=w)
        return self.P.op("act", lambda e: e.activation(out=out, in_=in_, func=func, bias=bias, scale=scale, accum_out=accum_out), r=r, w=w)

    def copy(self, eng, out, in_, r=(), w=()):
        if eng == "act":
            return self.P.op("act", lambda e: e.copy(out=out, in_=in_), r=r, w=w)
        return self.P.op(eng, lambda e: e.tensor_copy(out=out, in_=in_), r=r, w=w)

    def tt(self, eng, out, in0, in1, op, r=(), w=()):
        return self.P.op(eng, lambda e: e.tensor_tensor(out=out, in0=in0, in1=in1, op=op), r=r, w=w)

    def ts(self, eng, out, in0, s1, op0, s2=None, op1=None, r=(), w=()):
        if op1 is None:
            return self.P.op(eng, lambda e: e.tensor_scalar(out=out, in0=in0, scalar1=s1, scalar2=None, op0=op0), r=r, w=w)
        return self.P.op(eng, lambda e: e.tensor_scalar(out=out, in0=in0, scalar1=s1, scalar2=s2, op0=op0, op1=op1), r=r, w=w)

    def stt(self, eng, out, in0, scalar, in1, op0, op1, r=(), w=()):
        return self.P.op(eng, lambda e: e.scalar_tensor_tensor(out=out, in0=in0, scalar=scalar, in1=in1, op0=op0, op1=op1), r=r, w=w)

    def memset(self, eng, ap, val, r=(), w=()):
        return self.P.op(eng, lambda e: e.memset(ap, val), r=r, w=w)

    def dma(self, eng, semkey, out, in_, r=(), w=()):
        return self.P.dma(eng, semkey, lambda e: e.dma_start(out=out, in_=in_), r=r, w=w)


class PsumPool:
    def __init__(self, P):
        self.P = P
        self.banks = [P.ps("pb%d" % i, [128, 512], F32) for i in range(8)]
        self.rr = {}

    def bank(self, b):
        return self.banks[b], [("pb", b)]

    def rot(self, name, choices):
        i = self.rr.get(name, 0)
        self.rr[name] = i + 1
        return choices[i % len(choices)]

    def small(self, name, slots):
        b = self.rot(name, slots)
        return self.banks[b][:, 0:128], [("pb", b)]


def _consts_f32():
    p = np.arange(128)[:, None]
    f = np.arange(128)[None, :]
    same = (p // 64) == (f // 64)
    c = {}
    c["IDN"] = (p == f).astype(np.float32)
    c["LT1"] = (same & (p <= f)).astype(np.float32)
    c["SC"] = same.astype(np.float32)
    c["MLS"] = (same & (p > f)).astype(np.float32)
    c["MUI"] = (same & (f >= p)).astype(np.float32)
    c["ONES"] = np.ones((128, 128), np.float32)
    rp = np.zeros((128, 128), np.float32)
    for dst in range(64):
        if dst < 32:
            rp[dst + 32, dst] = -1.0
        else:
            rp[dst - 32, dst] = 1.0
    c["RP"] = rp
    misc = np.zeros((128, 128), np.float32)
    misc[:, 0] = (np.arange(128) < 64)
    misc[:, 1] = (np.arange(128) >= 64)
    half = 32
    inv_freq = (np.float32(10000.0) ** (-np.arange(half, dtype=np.float32) / np.float32(half))).astype(np.float32)
    misc[:, 2] = np.tile(inv_freq, 4)
    c["MISC"] = misc
    names = ["IDN", "LT1", "SC", "MLS", "MUI", "ONES", "RP", "MISC"]
    return np.concatenate([c[n] for n in names], axis=1), names


NCOL_A = 706
CQ, CK, CV, CAQ0, CAQ1, CAK0, CAK1, CAV = 0, 128, 256, 322, 386, 450, 514, 578


def build_phase_a(S):
    NT = S // TL
    NS = NT // 2
    NB = S // 128
    SQ = S // 2
    nc = bass.Bass("TRN2", target_bir_lowering=False)
    dr = lambda name, shape, dt, kind="ExternalInput": nc.dram_tensor(name, list(shape), dt, kind=kind).ap()
    xT_d = dr("xT", [D, S], F32)
    xTo_d = dr("xT_own", [D, SQ], F32)
    ccol_d = dr("c_col", [128, 8], F32)
    wada_d = dr("w_ada_a", [D, 2048], F32)
    bada_d = dr("b_ada_col", [128, 16], F32)
    wa_d = dr("w_a", [D, NCOL_A], F32)
    conv_d = dr("conv_col", [128, 12], F32)
    headc_d = dr("headc", [128, 4], F32)
    lam_d = dr("lam4", [128, 256], F32)
    pos_d = dr("pos", [1, S], I32)
    poso_d = dr("pos_own", [1, SQ], I32)
    dmask_d = dr("dmask", [128, 2 * TL], F32)
    cst_d = dr("consts", [128, 8 * 128], F32)
    odn_d = dr("o_dn", [S, 64], F32, kind="ExternalOutput")
    oda_d = dr("o_da", [SQ, 128], F32, kind="ExternalOutput")

    P = Prog(nc)
    E = Em(P)
    PS = PsumPool(P)
    BIG = [6, 7, 0, 1]
    SMALL = BIG

    cst = P.sb("cst", [128, 8 * 128], F32)
    cs = lambda i: cst[:, i * 128:(i + 1) * 128]
    IDN, LT1, SC, MLS, MUI, ONES, RPm, MISC = [cs(i) for i in range(8)]
    CI = MISC[:, 0:2]
    INVF = MISC[0:64, 2:3]
    KT = [P.sb("KT%d" % m, [65, S], BF16) for m in range(2)]
    VA = P.sb("VA", [128, NB, 129], BF16)
    W = P.sb("W", [128, 8, NCOL_A], BF16)
    xs = P.sb("xs", [128, 8, TL], F32)
    uT = P.sb("uT", [128, 8, TL], BF16)
    dmf = P.sb("dmf", [128, 2 * TL], F32)
    dmask = P.sb("dmask", [128, 2, TL], BF16)
    ccol = P.sb("ccol", [128, 8], F32)
    silc = P.sb("silc", [128, 8], F32)
    bada = P.sb("bada", [128, 16], F32)
    shiftc = P.sb("shiftc", [128, 8], F32)
    scale1 = P.sb("scale1", [128, 8], F32)
    convc = P.sb("convc", [128, 12], F32)
    headc = P.sb("headc", [128, 4], F32)
    hc2 = P.sb("hc2", [128, 4], F32)
    lam4 = P.sb("lam4", [128, 256], F32)
    lamt = P.sb("lamt", [128, 128], F32)
    lams = P.sb("lams", [128, 4], F32)
    wst = P.sb("wst", [128, 8, 512], F32)
    pre = [P.sb("pre%d" % i, [128, TL + 3], F32) for i in range(3)]
    cacc = P.sb("cacc", [128, TL], F32)
    sil = [P.sb("sil%d" % i, [128, TL], F32) for i in range(2)]
    vbaT = P.sb("vbaT", [128, TL], F32)
    sq = P.sb("sq", [128, TL], F32)
    rn = P.sb("rn", [128, TL], F32)
    QnT = P.sb("QnT", [128, TL], F32)
    KnT = P.sb("KnT", [128, TL], F32)
    posi = P.sb("posi", [64, TL], I32)
    rsc = [P.sb("rsc%d" % i, [64, TL], F32) for i in range(4)]
    sinT = P.sb("sinT", [64, TL], F32)
    cosT = P.sb("cosT", [64, TL], F32)
    xk = P.sb("xk", [64, TL], F32)
    rt1 = P.sb("rt1", [64, TL], F32)
    rt2 = P.sb("rt2", [64, TL], F32)
    sqk = P.sb("sqk", [64, TL], F32)
    kmax = P.sb("kmax", [65, 2], F32)
    kmt = P.sb("kmt", [65, 2], F32)
    mqt = P.sb("mqt", [65, TL], F32)
    QT = [P.sb("QT%d" % m, [65, TL], BF16) for m in range(2)]
    PT = [P.sb("PT%d" % i, [128, 2 * TL], BF16) for i in range(2)]
    NBT = TL // 128
    Kn = P.sb("Kn", [128, NBT, 128], F32)
    VBA = P.sb("VBA", [128, NBT, 66], F32)
    beta = P.sb("beta", [128, NBT], F32)
    gsc = [P.sb("gsc%d" % i, [128, NBT], F32) for i in range(4)]
    gg = P.sb("gg", [128, NBT], F32)
    names_bt = ["Gb", "dd", "tmn", "tmx", "E1", "E2", "Am", "Bm", "Y0", "Y0T", "Y1", "Y1T", "Rm", "kb", "kdec", "ecB", "QdT", "qkT", "WT"]
    bt = {n: P.sb("bt_" + n, [128, 128], F32) for n in names_bt}
    cumc = P.sb("cumc", [128, 2], F32)
    sc3 = P.sb("sc3", [128, 4], F32)
    cdt = P.sb("cdt", [128, 2], F32)
    bv = P.sb("bv", [128, 64], F32)
    Ub = P.sb("Ub", [128, 64], F32)
    ut = P.sb("ut", [128, 64], F32)
    Sst = P.sb("Sst", [128, 64], F32)
    osb = P.sb("osb", [128, NBT, 64], F32)
    rec = P.sb("rec", [128, 8], F32)
    o1t = P.sb("o1t", [128, 128], F32)
    odasb = P.sb("odasb", [128, 2, 128], F32)

    E.dma("sp", "cst", cst[:], cst_d[:], w=["cst"])
    E.dma("pool", "W", W[:], wa_d.rearrange("(k p) c -> p k c", p=128), w=["W"])
    E.dma("sp", "ccol", ccol[:], ccol_d[:], w=["ccol"])
    E.dma("sp", "bada", bada[:], bada_d[:], w=["bada"])
    E.dma("sp", "convc", convc[:], conv_d[:], w=["convc"])
    E.dma("sp", "headc", headc[:], headc_d[:], w=["headc"])
    E.dma("sp", "lam4", lam4[:], lam_d[:], w=["lam4"])
    E.dma("sp", "dmf", dmf[:], dmask_d[:], w=["dmf"])
    E.copy("dve", dmask[:].rearrange("p a b -> p (a b)"), dmf[:], r=["dmf"], w=["dmask"])
    E.memset("pool", VA[:, :, 128:129], 1.0, w=["VAones"])
    for m in range(2):
        E.memset("pool", KT[m][64:65, :], 1.0, w=[("KTones", m)])
    E.memset("dve", kmax[:], 0.0, w=["kmax"])
    E.memset("dve", Sst[:], 0.0, w=["S"])
    for i in range(3):
        E.memset("pool", pre[i][:], 0.0, w=[("pre", i)])
    E.act(hc2[:, 0:1], headc[:, 0:1], AF.Exp, r=["headc"], w=["hc2a"])
    E.ts("dve", hc2[:, 0:1], hc2[:, 0:1], -1.0, ALU.mult, r=["hc2a"], w=["hc2a"])
    E.copy("dve", hc2[:, 1:2], headc[:, 1:2], r=["headc"], w=["hc2b"])
    E.copy("dve", hc2[:, 3:4], headc[:, 2:3], r=["headc"], w=["hc2d"])
    E.tt("dve", lamt[:, 0:64], lam4[:, 0:64], lam4[:, 64:128], ALU.mult, r=["lam4"], w=["lamt0"])
    E.tt("dve", lamt[:, 64:128], lam4[:, 128:192], lam4[:, 192:256], ALU.mult, r=["lam4"], w=["lamt1"])
    P.op("dve", lambda e: e.reduce_sum(out=lams[:, 0:1], in_=lamt[:, 0:64], axis=AX.X), r=["lamt0"], w=["lams0"])
    P.op("dve", lambda e: e.reduce_sum(out=lams[:, 1:2], in_=lamt[:, 64:128], axis=AX.X), r=["lamt1"], w=["lams1"])
    E.act(lams[:, 2:4], lams[:, 0:2], AF.Exp, r=["lams0", "lams1"], w=["lams2"])
    E.tt("dve", lams[:, 0:1], lams[:, 3:4], lams[:, 2:3], ALU.subtract, r=["lams2"], w=["lams0"])
    E.tt("dve", hc2[:, 2:3], lams[:, 0:1], headc[:, 3:4], ALU.subtract, r=["lams0", "headc"], w=["hc2c"])
    NEGA, DTB, NLAM, HALF = hc2[:, 0:1], hc2[:, 1:2], hc2[:, 2:3], hc2[:, 3:4]

    E.act(silc[:], ccol[:], AF.Silu, r=["ccol"], w=["silc"])
    mcol, mkeys = PS.bank(6)
    for pc in range(4):
        E.dma("sp", "wst", wst[:], wada_d.rearrange("(k p) c -> p k c", p=128)[:, :, pc * 512:(pc + 1) * 512], w=["wst"])
        for jj in range(4):
            j = pc * 4 + jj
            for k in range(8):
                E.mm(mcol[:, j:j + 1], wst[:, k, jj * 128:(jj + 1) * 128], silc[:, k:k + 1], start=(k == 0), stop=(k == 7),
                     r=["wst", "silc", "cst"], w=mkeys)
    E.tt("dve", shiftc[:], mcol[:, 0:8], bada[:, 0:8], ALU.add, r=mkeys + ["bada"], w=["shiftc"])
    E.stt("dve", scale1[:], mcol[:, 8:16], 1.0, bada[:, 8:16], ALU.add, ALU.add, r=mkeys + ["bada"], w=["scale1"])

    def load_modulate(src_d, t0):
        E.dma("sp", "xs", xs[:], src_d.rearrange("(k p) t -> p k t", p=128)[:, :, t0:t0 + TL], w=["xs"])
        for k in range(8):
            if k % 2 == 0:
                E.act(uT[:, k, :], xs[:, k, :], AF.Identity, bias=shiftc[:, k:k + 1], scale=scale1[:, k:k + 1],
                      r=["xs", "shiftc", "scale1"], w=[("uT", k)])
            else:
                E.ts("dve", uT[:, k, :], xs[:, k, :], scale1[:, k:k + 1], ALU.mult, shiftc[:, k:k + 1], ALU.add,
                     r=["xs", "shiftc", "scale1"], w=[("uT", k)])

    UTK = [("uT", k) for k in range(8)]

    def proj_fm(col0, M):
        b = PS.rot("sm", BIG)
        bank, keys = PS.bank(b)
        for k in range(8):
            E.mm(bank[0:M, 0:TL], W[:, k, col0:col0 + M], uT[:, k, :], start=(k == 0), stop=(k == 7), r=["W"] + UTK, w=keys)
        return bank, keys

    def rope_tables(posd, t0):
        E.dma("sp", "posi", posi[:], posd[0:1, t0:t0 + TL].partition_broadcast(64), w=["posi"])
        a, t, n, r_ = rsc
        E.copy("dve", a[:], posi[:], r=["posi"], w=["rs0"])
        E.ts("dve", a[:], a[:], INVF, ALU.mult, r=["rs0", "cst"], w=["rs0"])
        E.ts("dve", t[:], a[:], 1.0 / TWO_PI, ALU.mult, MAGIC, ALU.add, r=["rs0"], w=["rs1"])
        E.ts("dve", n[:], t[:], MAGIC, ALU.subtract, r=["rs1"], w=["rs2"])
        E.stt("dve", r_[:], n[:], -C1, a[:], ALU.mult, ALU.add, r=["rs2", "rs0"], w=["rs3"])
        E.stt("dve", r_[:], n[:], -C2, r_[:], ALU.mult, ALU.add, r=["rs2", "rs3"], w=["rs3"])
        E.act(sinT[:], r_[:], AF.Sin, r=["rs3"], w=["sinT"])
        E.ts("dve", t[:], r_[:], np.pi / 2, ALU.add, r=["rs3"], w=["rs1"])
        E.ts("dve", n[:], t[:], np.pi, ALU.is_gt, r=["rs1"], w=["rs2"])
        E.stt("dve", t[:], n[:], -TWO_PI, t[:], ALU.mult, ALU.add, r=["rs2", "rs1"], w=["rs1"])
        E.act(cosT[:], t[:], AF.Sin, r=["rs1"], w=["cosT"])

    def rope_apply(bank, keys, out_bf, out_keys, prescale):
        E.copy("act", xk[:], bank[0:64, 0:TL], r=keys, w=["xk"])
        b2 = PS.rot("sm", BIG)
        bank2, keys2 = PS.bank(b2)
        E.mm(bank2[0:64, 0:TL], RPm[0:64, 0:64], xk[:], r=["xk", "cst"], w=keys2)
        E.tt("pool", rt1[:], xk[:], cosT[:], ALU.mult, r=["xk", "cosT"], w=["rt1"])
        E.tt("dve", rt2[:], bank2[0:64, 0:TL], sinT[:], ALU.mult, r=keys2 + ["sinT"], w=["rt2"])
        if prescale != 1.0:
            E.stt("dve", out_bf, rt1[:], prescale, rt2[:], ALU.mult, ALU.add, r=["rt1", "rt2"], w=out_keys)
        else:
            E.tt("dve", out_bf, rt1[:], rt2[:], ALU.add, r=["rt1", "rt2"], w=out_keys)
        E.tt("pool", sqk[:], xk[:], xk[:], ALU.mult, r=["xk"], w=["sqk"])
        b3 = PS.rot("sm", BIG)
        bank3, keys3 = PS.bank(b3)
        E.mm(bank3[0:65, 0:TL], ONES[0:64, 0:65], sqk[:], r=["sqk", "cst"], w=keys3)
        return bank3, keys3

    def conv_silu(ci, bank, keys, M, out_ap, out_key, silu_rows):
        pr = pre[ci]
        E.copy("act", pr[0:M, 3:TL + 3], bank[0:M, 0:TL], r=keys, w=[("pre", ci)])
        cw = lambda j: convc[0:M, ci * 4 + j:ci * 4 + j + 1]
        E.ts("dve", cacc[0:M, :], pr[0:M, 3:TL + 3], cw(3), ALU.mult, r=[("pre", ci), "convc"], w=["cacc"])
        for j in (2, 1, 0):
            E.stt("dve", cacc[0:M, :], pr[0:M, j:j + TL], cw(j), cacc[0:M, :], ALU.mult, ALU.add, r=[("pre", ci), "convc", "cacc"], w=["cacc"])
        E.copy("act", pr[0:M, 0:3], pr[0:M, TL:TL + 3], r=[("pre", ci), "cacc"], w=[("pre", ci)])
        E.act(out_ap[0:silu_rows, :], cacc[0:silu_rows, :], AF.Silu, r=["cacc"], w=[out_key])
        if silu_rows < M:
            E.copy("dve", out_ap[64:M, :], cacc[64:M, :], r=["cacc"], w=[out_key + "_x"])

    def l2norm(src, src_key, dst, dst_key, post):
        E.tt("pool", sq[:], src[:], src[:], ALU.mult, r=[src_key], w=["sq"])
        b = PS.rot("sm", BIG)
        bank, keys = PS.bank(b)
        E.mm(bank[:, 0:TL], ONES, sq[:], r=["sq", "cst"], w=keys)
        E.act(rn[:], bank[:, 0:TL], AF.Sqrt, bias=EPS_L2, r=keys, w=["rn"])
        P.op("dve", lambda e: e.reciprocal(out=rn[:], in_=rn[:]), r=["rn"], w=["rn"])
        E.stt("dve", dst[:], src[:], post, rn[:], ALU.mult, ALU.mult, r=[src_key, "rn"], w=[dst_key])

    def dn_tile(t):
        for b in range(NBT):
            bs = slice(b * 128, (b + 1) * 128)
            pt, pk = PS.small("sm", SMALL)
            E.tr(pt, KnT[:, bs], IDN, r=["KnT", "cst"], w=pk)
            E.copy("act", Kn[:, b, :], pt, r=pk, w=[("Kn", b)])
            pt2, pk2 = PS.small("sm", SMALL)
            E.tr(pt2[:, 0:66], vbaT[0:66, bs], IDN[0:66, 0:66], r=["vbaT", "vbaT_x", "cst"], w=pk2)
            E.copy("dve", VBA[:, b, :], pt2[:, 0:66], r=pk2, w=[("VBA", b)])
        vk = [("VBA", b) for b in range(NBT)]
        E.act(beta[:], VBA[:, :, 64], AF.Sigmoid, r=vk, w=["beta"])
        xg, ax, ee, ll = gsc
        E.ts("dve", xg[:], VBA[:, :, 65], DTB, ALU.add, r=vk + ["hc2b"], w=["xg"])
        E.stt("dve", ax[:], xg[:], -1.0, xg[:], ALU.mult, ALU.max, r=["xg"], w=["ax"])
        E.act(ee[:], ax[:], AF.Exp, scale=-1.0, r=["ax"], w=["ee"])
        E.act(ll[:], ee[:], AF.Ln, bias=1.0, r=["ee"], w=["ll"])
        E.stt("dve", xg[:], xg[:], 0.0, ll[:], ALU.max, ALU.add, r=["xg", "ll"], w=["xg"])
        E.ts("dve", gg[:], xg[:], NEGA, ALU.mult, r=["xg", "hc2a"], w=["gg"])
        for b in range(NBT):
            dn_block(t, b)
        E.dma("sp", "osb", odn_d.rearrange("(b p) d -> p b d", p=128)[:, t * NBT:(t + 1) * NBT, :], osb[:], r=["osb"], w=["odn_out"])

    def dn_block(t, b):
        bs = slice(b * 128, (b + 1) * 128)
        gcol = gg[:, b:b + 1]
        bcol = beta[:, b:b + 1]
        T = bt
        pc, pck = PS.small("sm", SMALL)
        E.mm(pc[:, 0:1], LT1, gcol, r=["gg", "cst"], w=pck)
        E.mm(pc[:, 1:2], SC, gcol, r=["gg", "cst"], w=pck)
        E.copy("act", cumc[:], pc[:, 0:2], r=pck, w=["cumc"])
        E.ts("dve", T["Gb"][:], ONES, gcol, ALU.mult, r=["gg", "cst"], w=["Gb"])
        pB, pBk = PS.small("sm", SMALL)
        E.mm(pB, T["Gb"][:], LT1, r=["Gb", "cst"], w=pBk)
        pD, pDk = PS.small("sm", SMALL)
        E.mm(pD[:, 0:2], T["Gb"][:], CI, r=["Gb", "cst"], w=pDk)
        E.act(cdt[:], pD[:, 0:2], AF.Exp, r=pDk, w=["cdt"])
        E.ts("dve", T["dd"][:], pB, cumc[:, 0:1], ALU.subtract, r=pBk + ["cumc"], w=["dd"])
        E.act(T["ecB"][:], pB, AF.Exp, r=pBk, w=["ecB"])
        E.ts("pool", T["tmn"][:], T["dd"][:], 0.0, ALU.min, r=["dd"], w=["tmn"])
        E.ts("pool", T["tmx"][:], T["dd"][:], 0.0, ALU.max, r=["dd"], w=["tmx"])
        E.act(T["E2"][:], T["tmn"][:], AF.Exp, r=["tmn"], w=["E2"])
        E.act(T["E1"][:], T["tmx"][:], AF.Exp, scale=-1.0, r=["tmx"], w=["E1"])
        E.tt("pool", T["E2"][:], T["E2"][:], MUI, ALU.mult, r=["E2", "cst"], w=["E2"])
        E.tt("pool", T["E1"][:], T["E1"][:], MLS, ALU.mult, r=["E1", "cst"], w=["E1"])
        pK, pKk = PS.small("sm", SMALL)
        E.mm(pK, KnT[:, bs], KnT[:, bs], r=["KnT"], w=pKk)
        pQ, pQk = PS.small("sm", SMALL)
        E.mm(pQ, KnT[:, bs], QnT[:, bs], r=["KnT", "QnT"], w=pQk)
        E.stt("dve", T["Am"][:], pK, bcol, T["E1"][:], ALU.mult, ALU.mult, r=pKk + ["beta", "E1"], w=["Am"])
        E.tt("dve", T["qkT"][:], pQ, T["E2"][:], ALU.mult, r=pQk + ["E2"], w=["qkT"])
        pT, pTk = PS.small("sm", SMALL)
        E.tr(pT, T["Am"][:], IDN, r=["Am", "cst"], w=pTk)
        E.copy("act", T["Bm"][:], pT, r=pTk, w=["Bm"])
        E.tt("dve", T["Rm"][:], IDN, pT, ALU.subtract, r=pTk + ["cst"], w=["Rm"])
        Y, YT, yk, ytk = T["Bm"], T["Am"], "Bm", "Am"
        nxt = [("Y0", "Y0T"), ("Y1", "Y1T")]
        for lev in range(5):
            nY, nYT = nxt[lev % 2]
            last = (lev == 4)
            if not last:
                p1, p1k = PS.small("sm", SMALL)
                E.mm(p1, YT[:], Y[:], r=[yk, ytk], w=p1k)
                E.copy("act", T[nY][:], p1, r=p1k, w=[nY])
            p2, p2k = PS.small("sm", SMALL)
            E.mm(p2, Y[:], YT[:], r=[yk, ytk], w=p2k)
            E.copy("dve", T[nYT][:], p2, r=p2k, w=[nYT])
            p3, p3k = PS.small("sm", SMALL)
            E.mm(p3, T[nYT][:], T["Rm"][:], r=[nYT, "Rm"], w=p3k)
            E.tt("dve", T["Rm"][:], T["Rm"][:], p3, ALU.add, r=p3k + ["Rm"], w=["Rm"])
            Y, YT, yk, ytk = T[nY], T[nYT], nY, nYT
        E.act(sc3[:, 0:1], cumc[:, 0:1], AF.Exp, r=["cumc"], w=["sc3a"])
        E.tt("dve", sc3[:, 1:2], sc3[:, 0:1], bcol, ALU.mult, r=["sc3a", "beta"], w=["sc3b"])
        E.act(sc3[:, 2:3], cumc[:, 0:1], AF.Exp, bias=cumc[:, 1:2], scale=-1.0, r=["cumc"], w=["sc3c"])
        E.ts("pool", T["kb"][:], Kn[:, b, :], sc3[:, 1:2], ALU.mult, r=[("Kn", b), "sc3b"], w=["kb"])
        E.ts("pool", T["kdec"][:], Kn[:, b, :], sc3[:, 2:3], ALU.mult, r=[("Kn", b), "sc3c"], w=["kdec"])
        E.ts("dve", bv[:], VBA[:, b, 0:64], bcol, ALU.mult, r=[("VBA", b), "beta"], w=["bv"])
        pU, pUk = PS.small("sm", SMALL)
        E.mm(pU[:, 0:64], T["Rm"][:], bv[:], r=["Rm", "bv"], w=pUk)
        E.copy("act", Ub[:], pU[:, 0:64], r=pUk, w=["Ub"])
        pW, pWk = PS.small("sm", SMALL)
        E.mm(pW, T["kb"][:], T["Rm"][:], r=["kb", "Rm"], w=pWk)
        E.copy("act", T["WT"][:], pW, r=pWk, w=["WT"])
        E.tt("pool", T["QdT"][:], QnT[:, bs], T["ecB"][:], ALU.mult, r=["QnT", "ecB"], w=["QdT"])
        for c in range(2):
            cs_ = slice(c * 64, (c + 1) * 64)
            p4, p4k = PS.small("sm", SMALL)
            E.mm(p4[cs_, 0:64], T["WT"][:, cs_], Sst[:], r=["WT", "S"], w=p4k)
            E.tt("dve", ut[cs_, :], Ub[cs_, :], p4[cs_, 0:64], ALU.subtract, r=p4k + ["Ub"], w=["ut"])
            p5, p5k = PS.small("sm", SMALL)
            E.mm(p5[cs_, 0:64], T["QdT"][:, cs_], Sst[:], start=True, stop=False, r=["QdT", "S"], w=p5k)
            E.mm(p5[cs_, 0:64], T["qkT"][cs_, cs_], ut[cs_, :], start=False, stop=True, r=["qkT", "ut"], w=p5k)
            E.copy("act", osb[cs_, b, :], p5[cs_, 0:64], r=p5k, w=["osb"])
            p6, p6k = PS.small("sm", SMALL)
            E.mm(p6[:, 0:64], T["kdec"][cs_, :], ut[cs_, :], r=["kdec", "ut"], w=p6k)
            E.stt("dve", Sst[:], Sst[:], cdt[:, c:c + 1], p6[:, 0:64], ALU.mult, ALU.add, r=p6k + ["S", "cdt"], w=["S"])

    def kv_tile(t):
        t0 = t * TL
        rope_tables(pos_d, t0)
        for m in range(2):
            bank, keys = proj_fm(CAK0 + 64 * m, 64)
            b3, k3 = rope_apply(bank, keys, KT[m][0:64, t0:t0 + TL], [("KT", m, t)], 1.0)
            P.op("dve", lambda e, b3=b3, m=m: e.reduce_max(out=kmt[:, m:m + 1], in_=b3[0:65, 0:TL], axis=AX.X), r=k3, w=[("kmt", m)])
            E.tt("dve", kmax[:, m:m + 1], kmax[:, m:m + 1], kmt[:, m:m + 1], ALU.max, r=[("kmt", m), "kmax"], w=["kmax"])
        for b in range(NBT):
            bs = slice(b * 128, (b + 1) * 128)
            pv, pvk = PS.small("sm", SMALL)
            for k in range(8):
                E.mm(pv, uT[:, k, bs], W[:, k, CAV:CAV + 128], start=(k == 0), stop=(k == 7), r=["W"] + UTK, w=pvk)
            E.copy("act", VA[:, t * NBT + b, 0:128], pv, r=pvk, w=[("VA", t * NBT + b)])

    def q_slot(s):
        t0 = s * TL
        load_modulate(xTo_d, t0)
        rope_tables(poso_d, t0)
        for m in range(2):
            bank, keys = proj_fm(CAQ0 + 64 * m, 64)
            E.copy("act", xk[:], bank[0:64, 0:TL], r=keys, w=["xk"])
            b2 = PS.rot("sm", BIG)
            bank2, keys2 = PS.bank(b2)
            E.mm(bank2[0:64, 0:TL], RPm[0:64, 0:64], xk[:], r=["xk", "cst"], w=keys2)
            E.tt("pool", rt1[:], xk[:], cosT[:], ALU.mult, r=["xk", "cosT"], w=["rt1"])
            E.tt("dve", rt2[:], bank2[0:64, 0:TL], sinT[:], ALU.mult, r=keys2 + ["sinT"], w=["rt2"])
            E.tt("dve", rt1[:], rt1[:], rt2[:], ALU.add, r=["rt1", "rt2"], w=["rt1"])
            E.ts("dve", QT[m][0:64, :], rt1[:], 0.125, ALU.mult, r=["rt1"], w=[("QT", m)])
            E.tt("pool", sqk[:], xk[:], xk[:], ALU.mult, r=["xk"], w=["sqk"])
            b3 = PS.rot("sm", BIG)
            bank3, keys3 = PS.bank(b3)
            E.mm(bank3[0:65, 0:TL], ONES[0:64, 0:65], sqk[:], r=["sqk", "cst"], w=keys3)
            E.ts("dve", mqt[64:65, :], bank3[64:65, 0:TL], kmax[64:65, m:m + 1], ALU.mult, r=keys3 + ["kmax"], w=["mqt"])
            E.act(mqt[64:65, :], mqt[64:65, :], AF.Sqrt, r=["mqt"], w=["mqt"])
            E.ts("dve", QT[m][64:65, :], mqt[64:65, :], -0.125, ALU.mult, r=["mqt"], w=[("QTb", m)])

    def attention(s):
        NKB = 4 * (s + 1)
        accs = []
        for a in range(4):
            m_, sub_ = a // 2, a % 2
            accs.append((PS.banks[2 + a][:, 0:129], [("pb", 2 + a)]))
        for kb in range(NKB):
            stb = kb % 2
            bank, keys = PS.bank(stb)
            kt = kb // NBT
            for m in range(2):
                E.mm(bank[:, m * TL:(m + 1) * TL], KT[m][0:65, kb * 128:(kb + 1) * 128], QT[m][0:65, :],
                     r=[("KT", m, kt), ("KTones", m), ("QT", m), ("QTb", m)], w=keys)
            pt = PT[kb % 2]
            ptk = ("PT", kb % 2)
            E.act(pt[:], bank[:, 0:2 * TL], AF.Exp, r=keys, w=[ptk])
            j = kb - (NKB - 4)
            if j >= 0:
                for m in range(2):
                    if j < 2:
                        E.stt("dve", pt[:, m * TL:(m + 1) * TL], dmask[:, j, :], HALF, pt[:, m * TL:(m + 1) * TL], ALU.max, ALU.mult,
                              r=[ptk, "dmask", "hc2d"], w=[ptk])
                    else:
                        E.stt("dve", pt[:, m * TL:(m + 1) * TL], dmask[:, j - 2, :], HALF, pt[:, m * TL:(m + 1) * TL], ALU.mult, ALU.mult,
                              r=[ptk, "dmask", "hc2d"], w=[ptk])
            for sub in range(2):
                last = 4 * s + 2 + sub
                if kb > last:
                    continue
                for m in range(2):
                    acc, ak = accs[m * 2 + sub]
                    E.mm(acc, pt[:, m * TL + sub * 128:m * TL + (sub + 1) * 128], VA[:, kb, :], start=(kb == 0), stop=(kb == last),
                         r=[ptk, ("VA", kb), "VAones"], w=ak)
        for sub in range(2):
            a0, a0k = accs[sub]
            a1, a1k = accs[2 + sub]
            P.op("dve", lambda e, a0=a0, sub=sub: e.reciprocal(out=rec[:, sub * 2:sub * 2 + 1], in_=a0[:, 128:129]), r=a0k, w=[("rec", sub, 0)])
            P.op("dve", lambda e, a1=a1, sub=sub: e.reciprocal(out=rec[:, sub * 2 + 1:sub * 2 + 2], in_=a1[:, 128:129]), r=a1k, w=[("rec", sub, 1)])
            E.tt("dve", rec[:, sub * 2 + 1:sub * 2 + 2], rec[:, sub * 2 + 1:sub * 2 + 2], NLAM, ALU.mult, r=[("rec", sub, 1), "hc2c"], w=[("rec", sub, 1)])
            E.ts("dve", o1t[:], a0[:, 0:128], rec[:, sub * 2:sub * 2 + 1], ALU.mult, r=a0k + [("rec", sub, 0)], w=["o1t"])
            E.stt("dve", odasb[:, sub, :], a1[:, 0:128], rec[:, sub * 2 + 1:sub * 2 + 2], o1t[:], ALU.mult, ALU.add,
                  r=a1k + [("rec", sub, 1), "o1t"], w=["odasb"])
        E.dma("sp", "odasb", oda_d.rearrange("(b p) d -> p b d", p=128)[:, s * 2:(s + 1) * 2, :], odasb[:], r=["odasb"], w=["oda_out"])

    for t in range(NT):
        load_modulate(xT_d, t * TL)
        bank, keys = proj_fm(CQ, 128)
        conv_silu(0, bank, keys, 128, sil[0], "sil0", 128)
        bank, keys = proj_fm(CK, 128)
        conv_silu(1, bank, keys, 128, sil[1], "sil1", 128)
        bank, keys = proj_fm(CV, 66)
        conv_silu(2, bank, keys, 66, vbaT, "vbaT", 64)
        l2norm(sil[0], "sil0", QnT, "QnT", 128.0 ** -0.5)
        l2norm(sil[1], "sil1", KnT, "KnT", 1.0)
        kv_tile(t)
        dn_tile(t)
        if t % 2 == 1:
            s = t // 2
            q_slot(s)
            attention(s)
    P.final_wait("sp")
    P.run()
    P.close()
    return nc


def host_inputs_a(inp, S):
    x = inp["x"][0, :S]
    SQ = S // 2
    NT = S // TL
    xT = np.ascontiguousarray(x.T)
    c = inp["c"][0]
    c_col = np.ascontiguousarray(c.reshape(8, 128).T)
    w_ada_a = np.ascontiguousarray(inp["w_ada"][0][:, 0:2048])
    b_ada_col = np.ascontiguousarray(inp["b_ada"][0][0:2048].reshape(16, 128).T)
    w_in = inp["w_in"][0]
    conv_w = inp["conv_w"][0]
    cst, _ = _consts_f32()
    pos = np.ascontiguousarray(inp["positions"][:, :S]).astype(np.int32)
    p = np.arange(128)[:, None]
    f = np.arange(TL)[None, :]
    dm = np.concatenate([((128 * j + p) <= f).astype(np.float32) for j in range(2)], axis=1)
    lam4 = np.concatenate([inp["lambda_q1"][0], inp["lambda_k1"][0], inp["lambda_q2"][0], inp["lambda_k2"][0]])[None, :]
    lam4 = np.ascontiguousarray(np.broadcast_to(lam4, (128, 256))).astype(np.float32)
    lam_init = 0.8 - 0.6 * np.exp(-0.3 * 0)
    maps = []
    for i in range(NCORES):
        h, half = i % 4, i // 4
        o_q, o_k, o_v, o_z, o_b, o_a, o_aq, o_ak, o_av = 0, 512, 1024, 1536, 2048, 2052, 2056, 2568, 3080
        cols = np.concatenate([
            o_q + h * 128 + np.arange(128), o_k + h * 128 + np.arange(128),
            o_v + h * 128 + half * 64 + np.arange(64), [o_b + h], [o_a + h],
            o_aq + h * 128 + np.arange(128), o_ak + h * 128 + np.arange(128), o_av + h * 128 + np.arange(128)])
        w_a = np.ascontiguousarray(w_in[:, cols])
        conv_col = np.zeros((128, 3, 4), np.float32)
        conv_col[:, 0, :] = conv_w[:, h * 128 + np.arange(128)].T
        conv_col[:, 1, :] = conv_w[:, 512 + h * 128 + np.arange(128)].T
        conv_col[0:64, 2, :] = conv_w[:, 1024 + h * 128 + half * 64 + np.arange(64)].T
        conv_col[64:66, 2, 3] = 1.0
        headc = np.zeros((128, 4), np.float32)
        headc[:, 0] = inp["dn_a_log"][0, h]
        headc[:, 1] = inp["dn_dt_bias"][0, h]
        headc[:, 2] = float(half)
        headc[:, 3] = lam_init
        own_tiles = [2 * s + half for s in range(NT // 2)]
        own_idx = np.concatenate([np.arange(t * TL, (t + 1) * TL) for t in own_tiles])
        maps.append({
            "xT": xT, "xT_own": np.ascontiguousarray(xT[:, own_idx]), "c_col": c_col, "w_ada_a": w_ada_a,
            "b_ada_col": b_ada_col, "w_a": w_a, "conv_col": conv_col.reshape(128, 12), "headc": headc, "lam4": lam4,
            "pos": pos, "pos_own": np.ascontiguousarray(pos[:, own_idx]), "dmask": dm, "consts": cst,
        })
    return maps


def gather_a(results, S):
    NT = S // TL
    o_dn = np.zeros((S, 512), np.float32)
    o_da = np.zeros((S, 512), np.float32)
    for i in range(NCORES):
        h, half = i % 4, i // 4
        o_dn[:, h * 128 + half * 64:h * 128 + half * 64 + 64] = results[i]["o_dn"]
        own_tiles = [2 * s + half for s in range(NT // 2)]
        own_idx = np.concatenate([np.arange(t * TL, (t + 1) * TL) for t in own_tiles])
        o_da[own_idx, h * 128:(h + 1) * 128] = results[i]["o_da"]
    return o_dn, o_da


ALPHA = 2.0 ** 0.25
LAM_INIT0 = 0.8 - 0.6 * 1.0
NE = 32


def build_phase_b(NTOK):
    TPC = NTOK // 128
    GT = min(8, TPC)
    NG = TPC // GT
    nc = bass.Bass("TRN2", target_bir_lowering=False)
    dr = lambda name, shape, dt, kind="ExternalInput": nc.dram_tensor(name, list(shape), dt, kind=kind).ap()
    x_d = dr("x_tok", [NTOK, D], F32)
    odn_d = dr("odn", [NTOK, 512], F32)
    oda_d = dr("oda", [NTOK, 512], F32)
    ccol_d = dr("c_col", [128, 8], F32)
    wada_d = dr("w_ada", [D, 6144], F32)
    badac_d = dr("b_ada_col", [128, 48], F32)
    badar_d = dr("b_ada_row", [1, 6144], F32)
    wb_d = dr("w_b", [D, 2560], F32)
    wdn_d = dr("w_dn", [512, D], F32)
    wda_d = dr("w_da", [512, D], F32)
    wo_d = dr("w_o", [D, D], F32)
    nrm_d = dr("normw", [1, 256], F32)
    ln_d = dr("lnp", [1, 4096], F32)
    wr_d = dr("w_router", [D, NE], F32)
    br_d = dr("b_router", [1, NE], F32)
    wgu_d = dr("w_gu", [NE, 4, D, 512], F32)
    bgu_d = dr("b_gu", [NE, 2048], F32)
    wd_d = dr("w_down", [NE, D, D], F32)
    bd_d = dr("b_down", [NE, D], F32)
    cst_d = dr("consts", [128, 8 * 128], F32)
    out_d = dr("out", [NTOK, D], F32, kind="ExternalOutput")
    x1_d = dr("x1_scr", [NTOK, D], F32, kind="Internal")
    ufT_d = dr("ufT_scr", [TPC, 128, 8 * 128], BF16, kind="Internal")

    P = Prog(nc)
    E = Em(P)
    PS = PsumPool(P)
    ALLB = list(range(8))
    PAIRS = [(0, 1), (2, 3), (4, 5), (6, 7)]

    def pb1():
        b = PS.rot("one", ALLB)
        return PS.banks[b], [("pb", b)]

    def pb2():
        a, b = PS.rot("two", PAIRS)
        return (PS.banks[a], PS.banks[b]), [("pb", a), ("pb", b)]

    cst = P.sb("cst", [128, 8 * 128], F32)
    IDN = cst[:, 0:128]
    ONES = cst[:, 5 * 128:6 * 128]
    idb = P.sb("idb", [128, 128], BF16)
    onesb = P.sb("onesb", [1, 128], BF16)
    ccol = P.sb("ccol", [128, 8], F32)
    silc = P.sb("silc", [128, 8], F32)
    silcb = P.sb("silcb", [128, 8, 128], F32)
    badac = P.sb("badac", [128, 48], F32)
    mcol = P.sb("mcol", [128, 48], F32)
    gateb = P.sb("gateb", [128, 2, D], F32)
    lnb = P.sb("lnb", [128, 4, D], F32)
    nrmb = P.sb("nrmb", [128, 256], F32)
    wr = P.sb("wr", [128, 8, NE], F32)
    brb = P.sb("brb", [128, NE], F32)
    bdsb = P.sb("bdsb", [NE, D], F32)
    RW = P.sb("RW", [128, TPC, NE], F32)
    st8 = P.sb("st8", [128, 16], F32)
    AR = 40 * 1024
    arena = P.sb("arena", [128, AR], F32)
    abf = arena[:].bitcast(BF16)

    def fview(off, n):
        return arena[:, off:off + n]

    def bview(off, n):
        return abf[:, 2 * off:2 * off + n]

    o = 0
    Wb = bview(o, 8 * 2560).rearrange("p (k c) -> p k c", k=8); o += 8 * 2560 // 2
    Wdn = bview(o, 4 * D).rearrange("p (k c) -> p k c", k=4); o += 4 * D // 2
    Wda = bview(o, 4 * D).rearrange("p (k c) -> p k c", k=4); o += 4 * D // 2
    Wo = bview(o, 8 * D).rearrange("p (k c) -> p k c", k=8); o += 8 * D // 2
    F = []
    for i in range(4):
        F.append(fview(o, D)); o += D
    xT32 = fview(o, D).rearrange("p (k c) -> p k c", k=8); o += D
    ufT32 = fview(o, D).rearrange("p (k c) -> p k c", k=8); o += D
    odn_t = fview(o, 512); o += 512
    oda_t = fview(o, 512); o += 512
    sqt = fview(o, 512); o += 512
    siluz = fview(o, 512); o += 512
    uT = bview(o, D).rearrange("p (k c) -> p k c", k=8); o += D // 2
    ufTb = bview(o, D); o += D // 2
    mb = bview(o, D); o += D // 2
    mT = bview(o, D).rearrange("p (k c) -> p k c", k=8); o += D // 2
    a12 = bview(o, D); o += D // 2
    a12T = bview(o, D).rearrange("p (k c) -> p k c", k=8); o += D // 2
    wst = fview(o, 8 * 512).rearrange("p (k c) -> p k c", k=8); o += 8 * 512
    assert o <= AR, o
    o = 0
    Wgu = []
    for i in range(2):
        Wgu.append(bview(o, 4 * 8 * 512).rearrange("p (g k c) -> p g k c", g=4, k=8)); o += 4 * 8 * 512 // 2
    Wd = bview(o, 8 * D).rearrange("p (k c) -> p k c", k=8); o += 8 * D // 2
    ufT = bview(o, 8 * GT * 128).rearrange("p (t k c) -> p t k c", t=GT, k=8); o += 8 * GT * 128 // 2
    acc = fview(o, GT * D).rearrange("p (t c) -> p t c", t=GT); o += GT * D
    bgu = bview(o, 2048); o += 1024
    G = []
    for i in range(4):
        G.append(fview(o, 256)); o += 256
    actv = bview(o, D); o += D // 2
    actT = bview(o, D).rearrange("p (k c) -> p k c", k=8); o += D // 2
    rwT = fview(o, 128); o += 128
    H = []
    for i in range(3):
        H.append(fview(o, D)); o += D
    assert o <= AR, o

    E.dma("sp", "cst", cst[:], cst_d[:], w=["cst"])
    E.copy("dve", idb[:], IDN, r=["cst"], w=["idb"])
    E.memset("dve", onesb[:], 1.0, w=["onesb"])
    E.dma("sp", "ccol", ccol[:], ccol_d[:], w=["ccol"])
    E.dma("sp", "badac", badac[:], badac_d[:], w=["badac"])
    E.dma("sp", "gateb0", gateb[:, 0, :], badar_d[0:1, 2048:3072].partition_broadcast(128), w=["gateb0"])
    E.dma("sp", "gateb1", gateb[:, 1, :], badar_d[0:1, 5120:6144].partition_broadcast(128), w=["gateb1"])
    E.dma("sp", "lnb", lnb[:].rearrange("p a b -> p (a b)"), ln_d[0:1, :].partition_broadcast(128), w=["lnb"])
    E.dma("sp", "nrmb", nrmb[:], nrm_d[0:1, :].partition_broadcast(128), w=["nrmb"])
    E.dma("sp", "wr", wr[:], wr_d.rearrange("(k p) e -> p k e", p=128), w=["wr"])
    E.dma("sp", "brb", brb[:], br_d[0:1, :].partition_broadcast(128), w=["brb"])
    E.dma("sp", "bdsb", bdsb[:], bd_d[:], w=["bdsb"])
    E.ts("dve", nrmb[:, 128:256], nrmb[:, 128:256], 1.0 - LAM_INIT0, ALU.mult, r=["nrmb"], w=["nrmb"])
    E.act(silc[:], ccol[:], AF.Silu, r=["ccol"], w=["silc"])
    for k in range(8):
        E.ts("dve", silcb[:, k, :], ONES, silc[:, k:k + 1], ALU.mult, r=["cst", "silc"], w=[("silcb", k)])
    SCB = [("silcb", k) for k in range(8)]
    for pc in range(12):
        mod, hf = pc // 2, pc % 2
        E.dma("sp", "wst", wst[:], wada_d.rearrange("(k p) c -> p k c", p=128)[:, :, pc * 512:(pc + 1) * 512], w=["wst"])
        if mod in (2, 5):
            gi = 0 if mod == 2 else 1
            bank, keys = pb1()
            for k in range(8):
                E.mm(bank[:, :], silcb[:, k, :], wst[:, k, :], start=(k == 0), stop=(k == 7), r=["wst"] + SCB, w=keys)
            E.tt("dve", gateb[:, gi, hf * 512:(hf + 1) * 512], gateb[:, gi, hf * 512:(hf + 1) * 512], bank[:, :], ALU.add,
                 r=keys + ["gateb%d" % gi], w=["gateb%d" % gi])
        else:
            bank, keys = pb1()
            for jj in range(4):
                for k in range(8):
                    E.mm(bank[:, jj:jj + 1], wst[:, k, jj * 128:(jj + 1) * 128], silc[:, k:k + 1], start=(k == 0), stop=(k == 7),
                         r=["wst", "silc"], w=keys)
            j0 = mod * 8 + hf * 4
            E.tt("dve", mcol[:, j0:j0 + 4], bank[:, 0:4], badac[:, j0:j0 + 4], ALU.add, r=keys + ["badac"], w=[("mcol", pc)])
    MC = [("mcol", pc) for pc in range(12)]
    E.ts("dve", mcol[:, 8:16], mcol[:, 8:16], 1.0, ALU.add, r=MC, w=MC)
    E.ts("dve", mcol[:, 32:40], mcol[:, 32:40], 1.0, ALU.add, r=MC, w=MC)
    E.dma("pool", "Wb", Wb, wb_d.rearrange("(k p) c -> p k c", p=128), r=["wst"], w=["Wb"])
    E.dma("pool", "Wdn", Wdn, wdn_d.rearrange("(k p) c -> p k c", p=128), w=["Wdn"])
    E.dma("pool", "Wda", Wda, wda_d.rearrange("(k p) c -> p k c", p=128), w=["Wda"])
    E.dma("pool", "Wo", Wo, wo_d.rearrange("(k p) c -> p k c", p=128), w=["Wo"])

    def transpose_mod(src, src_keys, dst_bf, dst_bf_key, sh0, sc0, dst32=None, dst32_key=None):
        (b0, b1), keys = pb2()
        for k in range(8):
            bk = b0 if k < 4 else b1
            E.tr(bk[:, (k % 4) * 128:(k % 4 + 1) * 128], src[:, k * 128:(k + 1) * 128], IDN, r=src_keys + ["cst"], w=keys)
        for k in range(8):
            bk = b0 if k < 4 else b1
            pin = bk[:, (k % 4) * 128:(k % 4 + 1) * 128]
            E.act(dst_bf[:, k, :], pin, AF.Identity, bias=mcol[:, sh0 + k:sh0 + k + 1], scale=mcol[:, sc0 + k:sc0 + k + 1],
                  r=keys + MC, w=[dst_bf_key])
            if dst32 is not None:
                E.ts("dve", dst32[:, k, :], pin, mcol[:, sc0 + k:sc0 + k + 1], ALU.mult, mcol[:, sh0 + k:sh0 + k + 1], ALU.add,
                     r=keys + MC, w=[dst32_key])

    def transpose_bf(src_bf, src_keys, dst, dst_key, nk):
        bank, keys = pb1()
        pb = bank[:].bitcast(BF16)
        for k in range(nk):
            E.tr(pb[:, k * 128:(k + 1) * 128], src_bf[:, k * 128:(k + 1) * 128], idb[:], r=src_keys + ["idb"], w=keys)
        E.copy("act", dst[:, 0:nk, :].rearrange("p k c -> p (k c)"), pb[:, 0:nk * 128], r=keys, w=[dst_key])

    def rmsnorm_heads(src, src_key, nw, out_bf, out_key, mul=None, mul_key=None):
        E.tt("dve", sqt, src, src, ALU.mult, r=[src_key], w=["sqt"])
        P.op("dve", lambda e: e.reduce_sum(out=st8[:, 0:4], in_=sqt.rearrange("p (h c) -> p h c", h=4), axis=AX.X), r=["sqt"], w=["st8a"])
        E.act(st8[:, 4:8], st8[:, 0:4], AF.Sqrt, bias=EPS_N, scale=1.0 / 128.0, r=["st8a"], w=["st8b"])
        P.op("dve", lambda e: e.reciprocal(out=st8[:, 4:8], in_=st8[:, 4:8]), r=["st8b"], w=["st8b"])
        for h in range(4):
            hs = slice(h * 128, (h + 1) * 128)
            if mul is None:
                E.stt("dve", out_bf[:, hs], src[:, hs], st8[:, 4 + h:5 + h], nw, ALU.mult, ALU.mult, r=[src_key, "st8b", "nrmb"], w=[out_key])
            else:
                E.stt("dve", sqt[:, hs], src[:, hs], st8[:, 4 + h:5 + h], nw, ALU.mult, ALU.mult, r=[src_key, "st8b", "nrmb", "sqt"], w=["sqt"])
        if mul is not None:
            E.tt("dve", out_bf, sqt, mul, ALU.mult, r=["sqt", mul_key], w=[out_key])

    def layer_norm(src, src_key, gi, dst, dst_key, tmp, tmp_key):
        P.op("dve", lambda e: e.reduce_sum(out=st8[:, 8:9], in_=src, axis=AX.X), r=[src_key], w=["st8c"])
        E.ts("dve", st8[:, 9:10], st8[:, 8:9], -1.0 / D, ALU.mult, r=["st8c"], w=["st8d"])
        E.ts("dve", tmp, src, st8[:, 9:10], ALU.add, r=[src_key, "st8d"], w=[tmp_key])
        E.act(dst, tmp, AF.Square, r=[tmp_key], w=[dst_key], accum_out=st8[:, 10:11])
        E.act(st8[:, 11:12], st8[:, 10:11], AF.Sqrt, bias=EPS_N, scale=1.0 / D, r=[dst_key], w=["st8e"])
        P.op("dve", lambda e: e.reciprocal(out=st8[:, 11:12], in_=st8[:, 11:12]), r=["st8e"], w=["st8e"])
        E.stt("dve", dst, tmp, st8[:, 11:12], lnb[:, gi, :], ALU.mult, ALU.mult, r=[tmp_key, "st8e", "lnb"], w=[dst_key])
        E.tt("dve", dst, dst, lnb[:, gi + 1, :], ALU.add, r=[dst_key, "lnb"], w=[dst_key])

    def stage1(t):
        rows = slice(t * 128, (t + 1) * 128)
        xt, sg, m1, r_ = F
        E.dma("sp", "xt", xt, x_d[rows, :], w=["xt"])
        E.dma("sp", "odn_t", odn_t, odn_d[rows, :], w=["odn_t"])
        E.dma("sp", "oda_t", oda_t, oda_d[rows, :], w=["oda_t"])
        transpose_mod(xt, ["xt"], uT, "uT", 0, 8)
        bank, keys = pb1()
        for k in range(8):
            E.mm(bank[:, :], uT[:, k, :], Wb[:, k, 0:512], start=(k == 0), stop=(k == 7), r=["uT", "Wb"], w=keys)
        E.act(siluz, bank[:, :], AF.Silu, r=keys, w=["siluz"])
        rmsnorm_heads(odn_t, "odn_t", nrmb[:, 0:128], a12[:, 0:512], "a1", mul=siluz, mul_key="siluz")
        rmsnorm_heads(oda_t, "oda_t", nrmb[:, 128:256], a12[:, 512:1024], "a2")
        transpose_bf(a12, ["a1", "a2"], a12T, "a12T", 8)
        for br in range(2):
            (g0, g1), gk = pb2()
            for hf, bk in enumerate((g0, g1)):
                c0 = 512 + br * 1024 + hf * 512
                for k in range(8):
                    E.mm(bk[:, :], uT[:, k, :], Wb[:, k, c0:c0 + 512], start=(k == 0), stop=(k == 7), r=["uT", "Wb"], w=gk)
            for hf, bk in enumerate((g0, g1)):
                E.act(sg[:, hf * 512:(hf + 1) * 512], bk[:, :], AF.Sigmoid, r=gk, w=["sg"])
            (y0, y1), yk = pb2()
            Wp, wkey = (Wdn, "Wdn") if br == 0 else (Wda, "Wda")
            for hf, bk in enumerate((y0, y1)):
                for kc in range(4):
                    E.mm(bk[:, :], a12T[:, br * 4 + kc, :], Wp[:, kc, hf * 512:(hf + 1) * 512], start=(kc == 0), stop=(kc == 3),
                         r=["a12T", wkey], w=yk)
            for hf, bk in enumerate((y0, y1)):
                hs = slice(hf * 512, (hf + 1) * 512)
                if br == 0:
                    E.tt("dve", m1[:, hs], sg[:, hs], bk[:, :], ALU.mult, r=yk + ["sg"], w=["m1"])
                else:
                    E.tt("dve", sg[:, hs], sg[:, hs], bk[:, :], ALU.mult, r=yk + ["sg"], w=["sg"])
        E.tt("dve", mb, m1, sg, ALU.add, r=["m1", "sg"], w=["mb"])
        transpose_bf(mb, ["mb"], mT, "mT", 8)
        (o0, o1), ok = pb2()
        for hf, bk in enumerate((o0, o1)):
            for k in range(8):
                E.mm(bk[:, :], mT[:, k, :], Wo[:, k, hf * 512:(hf + 1) * 512], start=(k == 0), stop=(k == 7), r=["mT", "Wo"], w=ok)
        for hf, bk in enumerate((o0, o1)):
            hs = slice(hf * 512, (hf + 1) * 512)
            E.tt("dve", r_[:, hs], bk[:, :], gateb[:, 0, hs], ALU.mult, r=ok + ["gateb0"], w=["r_"])
        E.stt("dve", r_, xt, ALPHA, r_, ALU.mult, ALU.add, r=["xt", "r_"], w=["r_"])
        layer_norm(r_, "r_", 0, m1, "m1", sg, "sg")
        E.dma("sp", "x1out", x1_d[rows, :], m1, r=["m1"], w=["x1d"])
        transpose_mod(m1, ["m1"], ufTb.rearrange("p (k c) -> p k c", k=8), "ufTb", 24, 32, dst32=ufT32, dst32_key="ufT32")
        E.dma("sp", "ufTout", ufT_d[t], ufTb, r=["ufTb"], w=["ufTd"])
        bank, keys = pb1()
        for k in range(8):
            E.mm(bank[:, 0:NE], ufT32[:, k, :], wr[:, k, :], start=(k == 0), stop=(k == 7), r=["ufT32", "wr"], w=keys)
        lg = sqt[:, 0:NE]
        E.tt("dve", lg, bank[:, 0:NE], brb[:], ALU.add, r=keys + ["brb", "sqt"], w=["sqt"])
        P.op("dve", lambda e: e.max(out=st8[:, 0:8], in_=lg), r=["sqt"], w=["st8a"])
        E.ts("dve", sqt[:, 32:64], lg, st8[:, 3:4], ALU.is_ge, r=["sqt", "st8a"], w=["sqt"])
        E.ts("dve", st8[:, 12:13], st8[:, 0:1], -1.0, ALU.mult, r=["st8a"], w=["st8f"])
        E.act(sqt[:, 64:96], lg, AF.Exp, bias=st8[:, 12:13], r=["sqt", "st8f"], w=["sqt"])
        E.tt("dve", sqt[:, 64:96], sqt[:, 64:96], sqt[:, 32:64], ALU.mult, r=["sqt"], w=["sqt"])
        P.op("dve", lambda e: e.reduce_sum(out=st8[:, 13:14], in_=sqt[:, 64:96], axis=AX.X), r=["sqt"], w=["st8g"])
        P.op("dve", lambda e: e.reciprocal(out=st8[:, 13:14], in_=st8[:, 13:14]), r=["st8g"], w=["st8g"])
        E.ts("dve", RW[:, t, :], sqt[:, 64:96], st8[:, 13:14], ALU.mult, r=["sqt", "st8g"], w=[("RW", t)])

    def barrier():
        toks = {}
        for k, st in P.res.items():
            for tk in ([st[0]] if st[0] is not None else []) + st[1]:
                key = id(tk[0])
                if key not in toks or toks[key][1] < tk[1]:
                    toks[key] = tk
        for eng in ENGS:
            P.ops[eng].append(([(s, v) for (s, v, e) in toks.values()], None, None))
            for (s, v, e) in toks.values():
                P.known[eng][id(s)] = max(P.known[eng].get(id(s), -1), v)

    def load_expert(e, first):
        wb_ = Wgu[e % 2]
        for cg in range(4):
            E.dma("pool", ("Wgu", e % 2, cg), wb_[:, cg, :, :], wgu_d[e, cg].rearrange("(k p) c -> p k c", p=128), w=[("Wgu", e % 2, cg)])

    def moe_group(g):
        for tt_ in range(GT):
            E.dma("sp", ("ufTin", tt_), ufT[:, tt_, :, :].rearrange("p k c -> p (k c)"), ufT_d[g * GT + tt_], r=["ufTd"], w=[("ufT", tt_)])
        for tt_ in range(GT):
            t = g * GT + tt_
            bank, keys = pb1()
            E.tr(bank[0:NE, 0:128], RW[:, t, :], IDN, r=[("RW", t), "cst"], w=keys)
            E.copy("act", rwT[0:NE, :], bank[0:NE, 0:128], r=keys, w=["rwT"])
            (a0, a1), ak = pb2()
            for hf, bk in enumerate((a0, a1)):
                E.mm(bk[:, :], rwT[0:NE, :], bdsb[:, hf * 512:(hf + 1) * 512], r=["rwT", "bdsb"], w=ak)
            for hf, bk in enumerate((a0, a1)):
                E.copy("act", acc[:, tt_, hf * 512:(hf + 1) * 512], bk[:, :], r=ak, w=[("acc", tt_)])
        load_expert(0, True)
        for e in range(NE):
            if e + 1 < NE:
                load_expert(e + 1, False)
            E.dma("pool", "Wd", Wd, wd_d[e].rearrange("(k p) c -> p k c", p=128), w=["Wd"])
            E.dma("pool", "bgu", bgu[0:1, :], bgu_d[e:e + 1, :], w=["bgu"])
            wb_ = Wgu[e % 2]
            for tt_ in range(GT):
                t = g * GT + tt_
                for cg in range(4):
                    bank, keys = pb1()
                    for k in range(8):
                        E.mm(bank[:, :], ufT[:, tt_, k, :], wb_[:, cg, k, :], start=(k == 0), stop=False,
                             r=[("ufT", tt_), ("Wgu", e % 2, cg)], w=keys)
                    E.mm(bank[:, :], onesb[0:1, :], bgu[0:1, cg * 512:(cg + 1) * 512], start=False, stop=True, r=["onesb", "bgu"], w=keys)
                    gl, sgm, lnn, t3 = G
                    E.ts("dve", gl, bank[:, 0:256], 7.0, ALU.min, r=keys, w=["gl"])
                    E.ts("dve", lnn, bank[:, 256:512], -7.0, ALU.max, 7.0, ALU.min, r=keys, w=["lnn"])
                    E.act(sgm, gl, AF.Sigmoid, scale=1.702, r=["gl"], w=["sgm"])
                    E.stt("dve", t3, lnn, 1.0, gl, ALU.add, ALU.mult, r=["lnn", "gl"], w=["t3"])
                    E.stt("dve", actv[:, cg * 256:(cg + 1) * 256], t3, RW[:, t, e:e + 1], sgm, ALU.mult, ALU.mult,
                          r=["t3", "sgm", ("RW", t)], w=[("actv", cg)])
                transpose_bf(actv, [("actv", cg) for cg in range(4)], actT, "actT", 8)
                (d0, d1), dk = pb2()
                for hf, bk in enumerate((d0, d1)):
                    for kc in range(8):
                        E.mm(bk[:, :], actT[:, kc, :], Wd[:, kc, hf * 512:(hf + 1) * 512], start=(kc == 0), stop=(kc == 7),
                             r=["actT", "Wd"], w=dk)
                for hf, bk in enumerate((d0, d1)):
                    hs = slice(hf * 512, (hf + 1) * 512)
                    E.tt("dve", acc[:, tt_, hs], acc[:, tt_, hs], bk[:, :], ALU.add, r=dk + [("acc", tt_)], w=[("acc", tt_)])
        for tt_ in range(GT):
            t = g * GT + tt_
            rows = slice(t * 128, (t + 1) * 128)
            x1t, r2, tmp = H
            E.dma("sp", "x1in", x1t, x1_d[rows, :], r=["x1d"], w=["x1t"])
            E.tt("dve", r2, acc[:, tt_, :], gateb[:, 1, :], ALU.mult, r=[("acc", tt_), "gateb1"], w=["r2"])
            E.stt("dve", r2, x1t, ALPHA, r2, ALU.mult, ALU.add, r=["x1t", "r2"], w=["r2"])
            layer_norm(r2, "r2", 2, x1t, "x1t", tmp, "tmp")
            E.dma("sp", "outd", out_d[rows, :], x1t, r=["x1t"], w=["outd"])

    for t in range(TPC):
        stage1(t)
    barrier()
    for g in range(NG):
        moe_group(g)
    P.final_wait("sp")
    P.run()
    P.close()
    return nc


def host_inputs_b(inp, S, o_dn, o_da):
    NTOK = S // NCORES
    x = inp["x"][0, :S]
    c = inp["c"][0]
    cst, _ = _consts_f32()
    wg = inp["w_gate_up"][0]
    idx = np.concatenate([np.concatenate([2 * (256 * cg + np.arange(256)), 2 * (256 * cg + np.arange(256)) + 1]) for cg in range(4)])
    w_gu = np.ascontiguousarray(wg[:, :, idx].reshape(NE, D, 4, 512).transpose(0, 2, 1, 3))
    b_gu = np.ascontiguousarray(inp["b_gate_up"][0][:, idx])
    w_in = inp["w_in"][0]
    w_b = np.ascontiguousarray(np.concatenate([w_in[:, 1536:2048], w_in[:, 3592:4616], w_in[:, 4616:5640]], axis=1))
    shared = {
        "c_col": np.ascontiguousarray(c.reshape(8, 128).T), "w_ada": np.ascontiguousarray(inp["w_ada"][0]),
        "b_ada_col": np.ascontiguousarray(inp["b_ada"][0].reshape(48, 128).T), "b_ada_row": np.ascontiguousarray(inp["b_ada"]),
        "w_b": w_b, "w_dn": np.ascontiguousarray(inp["w_dn_proj"][0]), "w_da": np.ascontiguousarray(inp["w_da_proj"][0]),
        "w_o": np.ascontiguousarray(inp["w_o"][0]),
        "normw": np.ascontiguousarray(np.concatenate([inp["dn_norm_w"][0], inp["da_norm_w"][0]])[None, :]),
        "lnp": np.ascontiguousarray(np.concatenate([inp["ln1_g"][0], inp["ln1_b"][0], inp["ln2_g"][0], inp["ln2_b"][0]])[None, :]),
        "w_router": np.ascontiguousarray(inp["w_router"][0]), "b_router": np.ascontiguousarray(inp["b_router"]),
        "w_gu": w_gu, "b_gu": b_gu, "w_down": np.ascontiguousarray(inp["w_down"][0]), "b_down": np.ascontiguousarray(inp["b_down"][0]),
        "consts": cst,
    }
    maps = []
    for i in range(NCORES):
        rows = slice(i * NTOK, (i + 1) * NTOK)
        m = dict(shared)
        m["x_tok"] = np.ascontiguousarray(x[rows])
        m["odn"] = np.ascontiguousarray(o_dn[rows])
        m["oda"] = np.ascontiguousarray(o_da[rows])
        maps.append(m)
    return maps


def kernel(**inp):
    inp = {k: np.asarray(v) for k, v in inp.items()}
    S = inp["x"].shape[1]
    nca = build_phase_a(S)
    ra = run_bass_kernel_spmd(nca, host_inputs_a(inp, S), core_ids=list(range(NCORES)))
    o_dn, o_da = gather_a(ra.results, S)
    ncb = build_phase_b(S // NCORES)
    rb = run_bass_kernel_spmd(ncb, host_inputs_b(inp, S, o_dn, o_da), core_ids=list(range(NCORES)))
    out = np.concatenate([rb.results[i]["out"] for i in range(NCORES)], axis=0)
    return out[None].astype(np.float32)
```

```python
import numpy as np
import ml_dtypes
import concourse.bass as bass
import concourse.mybir as mybir
from concourse.bass_utils import run_bass_kernel_spmd

F32 = mybir.dt.float32
BF16 = mybir.dt.bfloat16
I32 = mybir.dt.int32
ALU = mybir.AluOpType
AF = mybir.ActivationFunctionType
AX = mybir.AxisListType

ENGS = ("pe", "act", "dve", "pool", "sp")
import os as _os
DEBUG_TAGS = bool(_os.environ.get("KDEBUG"))
NCORES = 8
D = 1024
TL = 256
EPS_L2 = 1e-6
EPS_N = 1e-5
MAGIC = 12582912.0
TWO_PI = 6.283185307179586
C1 = 6.28125
C2 = TWO_PI - C1


class Prog:
    def __init__(self, nc, same_engine_sync=True):
        self.nc = nc
        self.ops = {e: [] for e in ENGS}
        self.cnt = {e: 0 for e in ENGS}
        self.esem = {}
        self.res = {}
        self.dsem = {}
        self.same = same_engine_sync
        self._stack = []
        self.known = {e: {} for e in ENGS}

    def _cm(self, cm):
        v = cm.__enter__()
        self._stack.append(cm)
        return v

    def sem(self, name):
        return self._cm(self.nc.semaphore(name))

    def sb(self, name, shape, dt):
        return self._cm(self.nc.sbuf_tensor("s_" + name, list(shape), dt))

    def ps(self, name, shape, dt):
        return self._cm(self.nc.psum_tensor(name, list(shape), dt))

    def close(self):
        while self._stack:
            self._stack.pop().__exit__(None, None, None)

    def _deps(self, eng, r, w, skip_same):
        toks = []
        for k in r:
            st = self.res.get(k)
            if st and st[0] is not None:
                toks.append(st[0])
        for k in w:
            st = self.res.get(k)
            if st:
                if st[0] is not None:
                    toks.append(st[0])
                toks.extend(st[1])
        best = {}
        for (s, v, e) in toks:
            if e == eng and (skip_same or not self.same):
                continue
            key = id(s)
            if key not in best or best[key][1] < v:
                best[key] = (s, v, e)
        out = []
        kn = self.known[eng]
        for key, (s, v, e) in best.items():
            if kn.get(key, -1) >= v:
                continue
            kn[key] = v
            out.append((s, v))
        return out

    def _commit(self, tok, r, w):
        for k in r:
            st = self.res.setdefault(k, [None, []])
            st[1].append(tok)
        for k in w:
            self.res[k] = [tok, []]

    def op(self, eng, fn, r=(), w=(), skip_same=False):
        pr = [k for k in r if isinstance(k, tuple) and k[0] == "pb"]
        if pr:
            r = [k for k in r if k not in pr]
            w = list(w) + [k for k in pr if k not in w]
        waits = self._deps(eng, r, w, skip_same)
        if eng not in self.esem:
            self.esem[eng] = self.sem("es_" + eng)
        self.cnt[eng] += 1
        tok = (self.esem[eng], self.cnt[eng], eng)
        self.ops[eng].append((waits, fn, (self.esem[eng], 1), self._tag()))
        self._commit(tok, r, w)
        return tok

    def _tag(self):
        if not DEBUG_TAGS:
            return None
        import sys
        f = sys._getframe(2)
        out = []
        while f is not None and len(out) < 4:
            if f.f_code.co_name not in ("op", "dma", "mm", "tr", "act", "copy", "tt", "ts", "stt", "memset", "<lambda>"):
                out.append("%s:%d" % (f.f_code.co_name, f.f_lineno))
            f = f.f_back
        return " < ".join(out)

    def dma(self, eng, semkey, fn, r=(), w=()):
        waits = self._deps(eng, r, w, False)
        if semkey not in self.dsem:
            self.dsem[semkey] = [self.sem("ds_%d" % len(self.dsem)), 0]
        d = self.dsem[semkey]
        d[1] += 16
        tok = (d[0], d[1], "dma")
        self.ops[eng].append((waits, fn, (d[0], 16), self._tag()))
        self._commit(tok, r, w)
        return tok

    def final_wait(self, eng="sp"):
        toks = {}
        for k, st in self.res.items():
            for t in ([st[0]] if st[0] is not None else []) + st[1]:
                key = id(t[0])
                if key not in toks or toks[key][1] < t[1]:
                    toks[key] = t
        self.ops[eng].append(([(s, v) for (s, v, e) in toks.values()], None, None, None))

    def run(self):
        nc = self.nc
        with nc.Block() as block:
            def replay(name):
                def f(e):
                    for waits, fn, inc, tag in self.ops[name]:
                        for (s, v) in waits:
                            e.wait_ge(s, v)
                        if fn is not None:
                            ins = fn(e)
                            ins.then_inc(inc[0], inc[1])
                            if tag:
                                ins.annotate(tag)
                return f
            block.tensor(replay("pe"))
            block.scalar(replay("act"))
            block.vector(replay("dve"))
            block.gpsimd(replay("pool"))
            block.sync(replay("sp"))


class Em:
    def __init__(self, P):
        self.P = P

    def mm(self, out, lhsT, rhs, start=True, stop=True, r=(), w=()):
        return self.P.op("pe", lambda e: e.matmul(out, lhsT=lhsT, rhs=rhs, start=start, stop=stop), r=r, w=w, skip_same=True)

    def tr(self, out, in_, ident, r=(), w=()):
        return self.P.op("pe", lambda e: e.transpose(out, in_, ident), r=r, w=w, skip_same=True)

    def act(self, out, in_, func, bias=0.0, scale=1.0, r=(), w=(), accum_out=None):
        if accum_out is None:
            return self.P.op("act", lambda e: e.activation(out=out, in_=in_, func=func, bias=bias, scale=scale), r=r, w=w)
        return self.P.op("act", lambda e: e.activation(out=out, in_=in_, func=func, bias=bias, scale=scale, accum_out=accum_out), r=r, w=w)

    def copy(self, eng, out, in_, r=(), w=()):
        if eng == "act":
            return self.P.op("act", lambda e: e.copy(out=out, in_=in_), r=r, w=w)
        return self.P.op(eng, lambda e: e.tensor_copy(out=out, in_=in_), r=r, w=w)

    def tt(self, eng, out, in0, in1, op, r=(), w=()):
        return self.P.op(eng, lambda e: e.tensor_tensor(out=out, in0=in0, in1=in1, op=op), r=r, w=w)

    def ts(self, eng, out, in0, s1, op0, s2=None, op1=None, r=(), w=()):
        if op1 is None:
            return self.P.op(eng, lambda e: e.tensor_scalar(out=out, in0=in0, scalar1=s1, scalar2=None, op0=op0), r=r, w=w)
        return self.P.op(eng, lambda e: e.tensor_scalar(out=out, in0=in0, scalar1=s1, scalar2=s2, op0=op0, op1=op1), r=r, w=w)

    def stt(self, eng, out, in0, scalar, in1, op0, op1, r=(), w=()):
        return self.P.op(eng, lambda e: e.scalar_tensor_tensor(out=out, in0=in0, scalar=scalar, in1=in1, op0=op0, op1=op1), r=r, w=w)

    def memset(self, eng, ap, val, r=(), w=()):
        return self.P.op(eng, lambda e: e.memset(ap, val), r=r, w=w)

    def dma(self, eng, semkey, out, in_, r=(), w=()):
        return self.P.dma(eng, semkey, lambda e: e.dma_start(out=out, in_=in_), r=r, w=w)


class PsumPool:
    def __init__(self, P):
        self.P = P
        self.banks = [P.ps("pb%d" % i, [128, 512], F32) for i in range(8)]
        self.rr = {}

    def bank(self, b):
        return self.banks[b], [("pb", b)]

    def rot(self, name, choices):
        i = self.rr.get(name, 0)
        self.rr[name] = i + 1
        return choices[i % len(choices)]

    def small(self, name, slots):
        b = self.rot(name, slots)
        return self.banks[b][:, 0:128], [("pb", b)]


def _consts_f32():
    p = np.arange(128)[:, None]
    f = np.arange(128)[None, :]
    same = (p // 64) == (f // 64)
    c = {}
    c["IDN"] = (p == f).astype(np.float32)
    c["LT1"] = (same & (p <= f)).astype(np.float32)
    c["SC"] = same.astype(np.float32)
    c["MLS"] = (same & (p > f)).astype(np.float32)
    c["MUI"] = (same & (f >= p)).astype(np.float32)
    c["ONES"] = np.ones((128, 128), np.float32)
    rp = np.zeros((128, 128), np.float32)
    for dst in range(64):
        if dst < 32:
            rp[dst + 32, dst] = -1.0
        else:
            rp[dst - 32, dst] = 1.0
    c["RP"] = rp
    misc = np.zeros((128, 128), np.float32)
    misc[:, 0] = (np.arange(128) < 64)
    misc[:, 1] = (np.arange(128) >= 64)
    half = 32
    inv_freq = (np.float32(10000.0) ** (-np.arange(half, dtype=np.float32) / np.float32(half))).astype(np.float32)
    misc[:, 2] = np.tile(inv_freq, 4)
    c["MISC"] = misc
    names = ["IDN", "LT1", "SC", "MLS", "MUI", "ONES", "RP", "MISC"]
    return np.concatenate([c[n] for n in names], axis=1), names


NCOL_A = 706
CQ, CK, CV, CAQ0, CAQ1, CAK0, CAK1, CAV = 0, 128, 256, 322, 386, 450, 514, 578


def build_phase_a(S):
    NT = S // TL
    NS = NT // 2
    NB = S // 128
    SQ = S // 2
    nc = bass.Bass("TRN2", target_bir_lowering=False)
    dr = lambda name, shape, dt, kind="ExternalInput": nc.dram_tensor(name, list(shape), dt, kind=kind).ap()
    xT_d = dr("xT", [D, S], F32)
    xTo_d = dr("xT_own", [D, SQ], F32)
    ccol_d = dr("c_col", [128, 8], F32)
    wada_d = dr("w_ada_a", [D, 2048], F32)
    bada_d = dr("b_ada_col", [128, 16], F32)
    wa_d = dr("w_a", [D, NCOL_A], F32)
    conv_d = dr("conv_col", [128, 12], F32)
    headc_d = dr("headc", [128, 4], F32)
    lam_d = dr("lam4", [128, 256], F32)
    pos_d = dr("pos", [1, S], I32)
    poso_d = dr("pos_own", [1, SQ], I32)
    dmask_d = dr("dmask", [128, 2 * TL], F32)
    cst_d = dr("consts", [128, 8 * 128], F32)
    odn_d = dr("o_dn", [S, 64], F32, kind="ExternalOutput")
    oda_d = dr("o_da", [SQ, 128], F32, kind="ExternalOutput")

    P = Prog(nc)
    E = Em(P)
    PS = PsumPool(P)
    BIG = [6, 7, 0, 1]
    SMALL = BIG

    cst = P.sb("cst", [128, 8 * 128], F32)
    cs = lambda i: cst[:, i * 128:(i + 1) * 128]
    IDN, LT1, SC, MLS, MUI, ONES, RPm, MISC = [cs(i) for i in range(8)]
    CI = MISC[:, 0:2]
    INVF = MISC[0:64, 2:3]
    KT = [P.sb("KT%d" % m, [65, S], BF16) for m in range(2)]
    VA = P.sb("VA", [128, NB, 129], BF16)
    W = P.sb("W", [128, 8, NCOL_A], BF16)
    xs = P.sb("xs", [128, 8, TL], F32)
    uT = P.sb("uT", [128, 8, TL], BF16)
    dmf = P.sb("dmf", [128, 2 * TL], F32)
    dmask = P.sb("dmask", [128, 2, TL], BF16)
    ccol = P.sb("ccol", [128, 8], F32)
    silc = P.sb("silc", [128, 8], F32)
    bada = P.sb("bada", [128, 16], F32)
    shiftc = P.sb("shiftc", [128, 8], F32)
    scale1 = P.sb("scale1", [128, 8], F32)
    convc = P.sb("convc", [128, 12], F32)
    headc = P.sb("headc", [128, 4], F32)
    hc2 = P.sb("hc2", [128, 4], F32)
    lam4 = P.sb("lam4", [128, 256], F32)
    lamt = P.sb("lamt", [128, 128], F32)
    lams = P.sb("lams", [128, 4], F32)
    wst = P.sb("wst", [128, 8, 512], F32)
    pre = [P.sb("pre%d" % i, [128, TL + 3], F32) for i in range(3)]
    cacc = P.sb("cacc", [128, TL], F32)
    sil = [P.sb("sil%d" % i, [128, TL], F32) for i in range(2)]
    vbaT = P.sb("vbaT", [128, TL], F32)
    sq = P.sb("sq", [128, TL], F32)
    rn = P.sb("rn", [128, TL], F32)
    QnT = P.sb("QnT", [128, TL], F32)
    KnT = P.sb("KnT", [128, TL], F32)
    posi = P.sb("posi", [64, TL], I32)
    rsc = [P.sb("rsc%d" % i, [64, TL], F32) for i in range(4)]
    sinT = P.sb("sinT", [64, TL], F32)
    cosT = P.sb("cosT", [64, TL], F32)
    xk = P.sb("xk", [64, TL], F32)
    rt1 = P.sb("rt1", [64, TL], F32)
    rt2 = P.sb("rt2", [64, TL], F32)
    sqk = P.sb("sqk", [64, TL], F32)
    kmax = P.sb("kmax", [65, 2], F32)
    kmt = P.sb("kmt", [65, 2], F32)
    mqt = P.sb("mqt", [65, TL], F32)
    QT = [P.sb("QT%d" % m, [65, TL], BF16) for m in range(2)]
    PT = [P.sb("PT%d" % i, [128, 2 * TL], BF16) for i in range(2)]
    NBT = TL // 128
    Kn = P.sb("Kn", [128, NBT, 128], F32)
    VBA = P.sb("VBA", [128, NBT, 66], F32)
    beta = P.sb("beta", [128, NBT], F32)
    gsc = [P.sb("gsc%d" % i, [128, NBT], F32) for i in range(4)]
    gg = P.sb("gg", [128, NBT], F32)
    names_bt = ["Gb", "dd", "tmn", "tmx", "E1", "E2", "Am", "Bm", "Y0", "Y0T", "Y1", "Y1T", "Rm", "kb", "kdec", "ecB", "QdT", "qkT", "WT"]
    bt = {n: P.sb("bt_" + n, [128, 128], F32) for n in names_bt}
    cumc = P.sb("cumc", [128, 2], F32)
    sc3 = P.sb("sc3", [128, 4], F32)
    cdt = P.sb("cdt", [128, 2], F32)
    bv = P.sb("bv", [128, 64], F32)
    Ub = P.sb("Ub", [128, 64], F32)
    ut = P.sb("ut", [128, 64], F32)
    Sst = P.sb("Sst", [128, 64], F32)
    osb = P.sb("osb", [128, NBT, 64], F32)
    rec = P.sb("rec", [128, 8], F32)
    o1t = P.sb("o1t", [128, 128], F32)
    odasb = P.sb("odasb", [128, 2, 128], F32)

    E.dma("sp", "cst", cst[:], cst_d[:], w=["cst"])
    E.dma("pool", "W", W[:], wa_d.rearrange("(k p) c -> p k c", p=128), w=["W"])
    E.dma("sp", "ccol", ccol[:], ccol_d[:], w=["ccol"])
    E.dma("sp", "bada", bada[:], bada_d[:], w=["bada"])
    E.dma("sp", "convc", convc[:], conv_d[:], w=["convc"])
    E.dma("sp", "headc", headc[:], headc_d[:], w=["headc"])
    E.dma("sp", "lam4", lam4[:], lam_d[:], w=["lam4"])
    E.dma("sp", "dmf", dmf[:], dmask_d[:], w=["dmf"])
    E.copy("dve", dmask[:].rearrange("p a b -> p (a b)"), dmf[:], r=["dmf"], w=["dmask"])
    E.memset("pool", VA[:, :, 128:129], 1.0, w=["VAones"])
    for m in range(2):
        E.memset("pool", KT[m][64:65, :], 1.0, w=[("KTones", m)])
    E.memset("dve", kmax[:], 0.0, w=["kmax"])
    E.memset("dve", Sst[:], 0.0, w=["S"])
    for i in range(3):
        E.memset("pool", pre[i][:], 0.0, w=[("pre", i)])
    E.act(hc2[:, 0:1], headc[:, 0:1], AF.Exp, r=["headc"], w=["hc2a"])
    E.ts("dve", hc2[:, 0:1], hc2[:, 0:1], -1.0, ALU.mult, r=["hc2a"], w=["hc2a"])
    E.copy("dve", hc2[:, 1:2], headc[:, 1:2], r=["headc"], w=["hc2b"])
    E.copy("dve", hc2[:, 3:4], headc[:, 2:3], r=["headc"], w=["hc2d"])
    E.tt("dve", lamt[:, 0:64], lam4[:, 0:64], lam4[:, 64:128], ALU.mult, r=["lam4"], w=["lamt0"])
    E.tt("dve", lamt[:, 64:128], lam4[:, 128:192], lam4[:, 192:256], ALU.mult, r=["lam4"], w=["lamt1"])
    P.op("dve", lambda e: e.reduce_sum(out=lams[:, 0:1], in_=lamt[:, 0:64], axis=AX.X), r=["lamt0"], w=["lams0"])
    P.op("dve", lambda e: e.reduce_sum(out=lams[:, 1:2], in_=lamt[:, 64:128], axis=AX.X), r=["lamt1"], w=["lams1"])
    E.act(lams[:, 2:4], lams[:, 0:2], AF.Exp, r=["lams0", "lams1"], w=["lams2"])
    E.tt("dve", lams[:, 0:1], lams[:, 3:4], lams[:, 2:3], ALU.subtract, r=["lams2"], w=["lams0"])
    E.tt("dve", hc2[:, 2:3], lams[:, 0:1], headc[:, 3:4], ALU.subtract, r=["lams0", "headc"], w=["hc2c"])
    NEGA, DTB, NLAM, HALF = hc2[:, 0:1], hc2[:, 1:2], hc2[:, 2:3], hc2[:, 3:4]

    E.act(silc[:], ccol[:], AF.Silu, r=["ccol"], w=["silc"])
    mcol, mkeys = PS.bank(6)
    for pc in range(4):
        E.dma("sp", "wst", wst[:], wada_d.rearrange("(k p) c -> p k c", p=128)[:, :, pc * 512:(pc + 1) * 512], w=["wst"])
        for jj in range(4):
            j = pc * 4 + jj
            for k in range(8):
                E.mm(mcol[:, j:j + 1], wst[:, k, jj * 128:(jj + 1) * 128], silc[:, k:k + 1], start=(k == 0), stop=(k == 7),
                     r=["wst", "silc", "cst"], w=mkeys)
    E.tt("dve", shiftc[:], mcol[:, 0:8], bada[:, 0:8], ALU.add, r=mkeys + ["bada"], w=["shiftc"])
    E.stt("dve", scale1[:], mcol[:, 8:16], 1.0, bada[:, 8:16], ALU.add, ALU.add, r=mkeys + ["bada"], w=["scale1"])

    QnT2 = [QnT, P.sb("QnT_b", [128, TL], F32)]
    KnT2 = [KnT, P.sb("KnT_b", [128, TL], F32)]
    vbaT2 = [vbaT, P.sb("vbaT_b", [128, TL], F32)]
    osb2 = [osb, P.sb("osb_b", [128, NBT, 64], F32)]
    SCR = ["Gb", "dd", "tmn", "tmx", "E1", "E2", "Am", "Bm", "Y0", "Y0T", "Y1", "Y1T", "Rm", "kb", "ecB"]
    OUTN = ["kdec", "QdT", "qkT", "WT"]
    scr = [bt] + [{n: P.sb("bt1_" + n, [128, 128], F32) for n in SCR}]
    outs = [[{n: (bt[n] if (tp == 0 and bb == 0) else P.sb("bo%d%d_%s" % (tp, bb, n), [128, 128], F32)) for n in OUTN}
             for bb in range(NBT)] for tp in range(2)]
    Ub4 = [[(Ub if (tp == 0 and bb == 0) else P.sb("Ub%d%d" % (tp, bb), [128, 64], F32)) for bb in range(NBT)] for tp in range(2)]
    cdt4 = [[(cdt if (tp == 0 and bb == 0) else P.sb("cdt%d%d" % (tp, bb), [128, 2], F32)) for bb in range(NBT)] for tp in range(2)]
    cumc2 = [cumc, P.sb("cumc_b", [128, 2], F32)]
    sc32 = [sc3, P.sb("sc3_b", [128, 4], F32)]
    bv2 = [bv, P.sb("bv_b", [128, 64], F32)]
    GEN = [4, 5, 6, 7]

    def gbank():
        b_ = PS.rot("gen", GEN)
        return PS.banks[b_], [("pb", b_)]

    def load_modulate(src_d, t0):
        E.dma("sp", "xs", xs[:], src_d.rearrange("(k p) t -> p k t", p=128)[:, :, t0:t0 + TL], w=["xs"])
        for k in range(8):
            if k % 2 == 0:
                E.act(uT[:, k, :], xs[:, k, :], AF.Identity, bias=shiftc[:, k:k + 1], scale=scale1[:, k:k + 1],
                      r=["xs", "shiftc", "scale1"], w=[("uT", k)])
            else:
                E.ts("dve", uT[:, k, :], xs[:, k, :], scale1[:, k:k + 1], ALU.mult, shiftc[:, k:k + 1], ALU.add,
                     r=["xs", "shiftc", "scale1"], w=[("uT", k)])

    UTK = [("uT", k) for k in range(8)]

    def proj_fm(col0, M):
        bank, keys = gbank()
        for k in range(8):
            E.mm(bank[0:M, 0:TL], W[:, k, col0:col0 + M], uT[:, k, :], start=(k == 0), stop=(k == 7), r=["W"] + UTK, w=keys)
        return bank, keys

    def rope_tables(posd, t0):
        E.dma("sp", "posi", posi[:], posd[0:1, t0:t0 + TL].partition_broadcast(64), w=["posi"])
        a, t, n, r_ = rsc
        E.copy("dve", a[:], posi[:], r=["posi"], w=["rs0"])
        E.ts("dve", a[:], a[:], INVF, ALU.mult, r=["rs0", "cst"], w=["rs0"])
        E.ts("dve", t[:], a[:], 1.0 / TWO_PI, ALU.mult, MAGIC, ALU.add, r=["rs0"], w=["rs1"])
        E.ts("dve", n[:], t[:], MAGIC, ALU.subtract, r=["rs1"], w=["rs2"])
        E.stt("dve", r_[:], n[:], -C1, a[:], ALU.mult, ALU.add, r=["rs2", "rs0"], w=["rs3"])
        E.stt("dve", r_[:], n[:], -C2, r_[:], ALU.mult, ALU.add, r=["rs2", "rs3"], w=["rs3"])
        E.act(sinT[:], r_[:], AF.Sin, r=["rs3"], w=["sinT"])
        E.ts("dve", t[:], r_[:], np.pi / 2, ALU.add, r=["rs3"], w=["rs1"])
        E.ts("dve", n[:], t[:], np.pi, ALU.is_gt, r=["rs1"], w=["rs2"])
        E.stt("dve", t[:], n[:], -TWO_PI, t[:], ALU.mult, ALU.add, r=["rs2", "rs1"], w=["rs1"])
        E.act(cosT[:], t[:], AF.Sin, r=["rs1"], w=["cosT"])

    def rope_core(bank, keys):
        E.copy("act", xk[:], bank[0:64, 0:TL], r=keys, w=["xk"])
        bank2, keys2 = gbank()
        E.mm(bank2[0:64, 0:TL], RPm[0:64, 0:64], xk[:], r=["xk", "cst"], w=keys2)
        E.tt("pool", rt1[:], xk[:], cosT[:], ALU.mult, r=["xk", "cosT"], w=["rt1"])
        E.tt("dve", rt2[:], bank2[0:64, 0:TL], sinT[:], ALU.mult, r=keys2 + ["sinT"], w=["rt2"])
        E.tt("pool", sqk[:], xk[:], xk[:], ALU.mult, r=["xk"], w=["sqk"])
        bank3, keys3 = gbank()
        E.mm(bank3[0:65, 0:TL], ONES[0:64, 0:65], sqk[:], r=["sqk", "cst"], w=keys3)
        return bank3, keys3

    def conv_silu(ci, bank, keys, M, out_ap, out_key, silu_rows):
        pr = pre[ci]
        E.copy("act", pr[0:M, 3:TL + 3], bank[0:M, 0:TL], r=keys, w=[("pre", ci)])
        cw = lambda j: convc[0:M, ci * 4 + j:ci * 4 + j + 1]
        E.ts("dve", cacc[0:M, :], pr[0:M, 3:TL + 3], cw(3), ALU.mult, r=[("pre", ci), "convc"], w=["cacc"])
        for j in (2, 1, 0):
            E.stt("dve", cacc[0:M, :], pr[0:M, j:j + TL], cw(j), cacc[0:M, :], ALU.mult, ALU.add, r=[("pre", ci), "convc", "cacc"], w=["cacc"])
        E.copy("act", pr[0:M, 0:3], pr[0:M, TL:TL + 3], r=[("pre", ci), "cacc"], w=[("pre", ci)])
        E.act(out_ap[0:silu_rows, :], cacc[0:silu_rows, :], AF.Silu, r=["cacc"], w=[out_key])
        if silu_rows < M:
            E.copy("dve", out_ap[64:M, :], cacc[64:M, :], r=["cacc"], w=[(out_key, "x")])

    def l2norm(src, src_key, dst, dst_key, post):
        E.tt("pool", sq[:], src[:], src[:], ALU.mult, r=[src_key], w=["sq"])
        bank, keys = gbank()
        E.mm(bank[:, 0:TL], ONES, sq[:], r=["sq", "cst"], w=keys)
        E.act(rn[:], bank[:, 0:TL], AF.Sqrt, bias=EPS_L2, r=keys, w=["rn"])
        P.op("dve", lambda e: e.reciprocal(out=rn[:], in_=rn[:]), r=["rn"], w=["rn"])
        E.stt("dve", dst[:], src[:], post, rn[:], ALU.mult, ALU.mult, r=[src_key, "rn"], w=[dst_key])

    def front(t):
        tp = t % 2
        load_modulate(xT_d, t * TL)
        yield
        bank, keys = proj_fm(CQ, 128)
        conv_silu(0, bank, keys, 128, sil[0], "sil0", 128)
        yield
        bank, keys = proj_fm(CK, 128)
        conv_silu(1, bank, keys, 128, sil[1], "sil1", 128)
        yield
        bank, keys = proj_fm(CV, 66)
        conv_silu(2, bank, keys, 66, vbaT2[tp], ("vbaT", tp), 64)
        yield
        l2norm(sil[0], "sil0", QnT2[tp], ("QnT", tp), 128.0 ** -0.5)
        yield
        l2norm(sil[1], "sil1", KnT2[tp], ("KnT", tp), 1.0)
        yield
        t0 = t * TL
        rope_tables(pos_d, t0)
        yield
        for m in range(2):
            bank, keys = proj_fm(CAK0 + 64 * m, 64)
            b3, k3 = rope_core(bank, keys)
            E.tt("dve", KT[m][0:64, t0:t0 + TL], rt1[:], rt2[:], ALU.add, r=["rt1", "rt2"], w=[("KT", m, t)])
            P.op("dve", lambda e, b3=b3, m=m: e.reduce_max(out=kmt[:, m:m + 1], in_=b3[0:65, 0:TL], axis=AX.X), r=k3, w=[("kmt", m)])
            E.tt("dve", kmax[:, m:m + 1], kmax[:, m:m + 1], kmt[:, m:m + 1], ALU.max, r=[("kmt", m), "kmax"], w=["kmax"])
            yield
        for b in range(NBT):
            bs = slice(b * 128, (b + 1) * 128)
            pv, pvk = gbank()
            for k in range(8):
                E.mm(pv[:, 0:128], uT[:, k, bs], W[:, k, CAV:CAV + 128], start=(k == 0), stop=(k == 7), r=["W"] + UTK, w=pvk)
            E.copy("act", VA[:, t * NBT + b, 0:128], pv[:, 0:128], r=pvk, w=[("VA", t * NBT + b)])
            yield

    def q_slot(s):
        t0 = s * TL
        load_modulate(xTo_d, t0)
        yield
        rope_tables(poso_d, t0)
        yield
        for m in range(2):
            bank, keys = proj_fm(CAQ0 + 64 * m, 64)
            bank3, keys3 = rope_core(bank, keys)
            E.tt("dve", rt1[:], rt1[:], rt2[:], ALU.add, r=["rt1", "rt2"], w=["rt1"])
            E.ts("dve", QT[m][0:64, :], rt1[:], 0.125, ALU.mult, r=["rt1"], w=[("QT", m)])
            E.ts("dve", mqt[64:65, :], bank3[64:65, 0:TL], kmax[64:65, m:m + 1], ALU.mult, r=keys3 + ["kmax"], w=["mqt"])
            E.act(mqt[64:65, :], mqt[64:65, :], AF.Sqrt, r=["mqt"], w=["mqt"])
            E.ts("dve", QT[m][64:65, :], mqt[64:65, :], -0.125, ALU.mult, r=["mqt"], w=[("QTb", m)])
            yield

    def dn_head(t):
        tp = t % 2
        for b in range(NBT):
            bs = slice(b * 128, (b + 1) * 128)
            pt, pk = gbank()
            E.tr(pt[:, 0:128], KnT2[tp][:, bs], IDN, r=[("KnT", tp), "cst"], w=pk)
            E.copy("act", Kn[:, b, :], pt[:, 0:128], r=pk, w=[("Kn", b)])
            pt2, pk2 = gbank()
            E.tr(pt2[:, 0:66], vbaT2[tp][0:66, bs], IDN[0:66, 0:66], r=[("vbaT", tp), (("vbaT", tp), "x"), "cst"], w=pk2)
            E.copy("dve", VBA[:, b, :], pt2[:, 0:66], r=pk2, w=[("VBA", b)])
        yield
        vk = [("VBA", b) for b in range(NBT)]
        E.act(beta[:], VBA[:, :, 64], AF.Sigmoid, r=vk, w=["beta"])
        xg, ax, ee, ll = gsc
        E.ts("dve", xg[:], VBA[:, :, 65], DTB, ALU.add, r=vk + ["hc2b"], w=["xg"])
        E.stt("dve", ax[:], xg[:], -1.0, xg[:], ALU.mult, ALU.max, r=["xg"], w=["ax"])
        E.act(ee[:], ax[:], AF.Exp, scale=-1.0, r=["ax"], w=["ee"])
        E.act(ll[:], ee[:], AF.Ln, bias=1.0, r=["ee"], w=["ll"])
        E.stt("dve", xg[:], xg[:], 0.0, ll[:], ALU.max, ALU.add, r=["xg", "ll"], w=["xg"])
        E.ts("dve", gg[:], xg[:], NEGA, ALU.mult, r=["xg", "hc2a"], w=["gg"])
        yield

    def dn_pre(t, b):
        tp = t % 2
        bs = slice(b * 128, (b + 1) * 128)
        gcol = gg[:, b:b + 1]
        bcol = beta[:, b:b + 1]
        T = dict(scr[b])
        K_ = lambda n: (n, b)
        O = outs[tp][b]
        OK_ = lambda n: (n, tp, b)
        cumc_, sc3_, bv_, Ub_, cdt_ = cumc2[b], sc32[b], bv2[b], Ub4[tp][b], cdt4[tp][b]
        QnT_, KnT_ = QnT2[tp], KnT2[tp]
        pc, pck = gbank()
        E.mm(pc[:, 0:1], LT1, gcol, r=["gg", "cst"], w=pck)
        E.mm(pc[:, 1:2], SC, gcol, r=["gg", "cst"], w=pck)
        E.copy("act", cumc_[:], pc[:, 0:2], r=pck, w=[K_("cumc")])
        E.ts("dve", T["Gb"][:], ONES, gcol, ALU.mult, r=["gg", "cst"], w=[K_("Gb")])
        yield
        pB_, pBk = gbank()
        pB = pB_[:, 0:128]
        E.mm(pB, T["Gb"][:], LT1, r=[K_("Gb"), "cst"], w=pBk)
        E.ts("dve", T["dd"][:], pB, cumc_[:, 0:1], ALU.subtract, r=pBk + [K_("cumc")], w=[K_("dd")])
        E.act(T["ecB"][:], pB, AF.Exp, r=pBk, w=[K_("ecB")])
        pD, pDk = gbank()
        E.mm(pD[:, 0:2], T["Gb"][:], CI, r=[K_("Gb"), "cst"], w=pDk)
        E.act(cdt_[:], pD[:, 0:2], AF.Exp, r=pDk, w=[OK_("cdt")])
        yield
        E.ts("pool", T["tmn"][:], T["dd"][:], 0.0, ALU.min, r=[K_("dd")], w=[K_("tmn")])
        E.ts("pool", T["tmx"][:], T["dd"][:], 0.0, ALU.max, r=[K_("dd")], w=[K_("tmx")])
        E.act(T["E2"][:], T["tmn"][:], AF.Exp, r=[K_("tmn")], w=[K_("E2")])
        E.act(T["E1"][:], T["tmx"][:], AF.Exp, scale=-1.0, r=[K_("tmx")], w=[K_("E1")])
        E.tt("pool", T["E2"][:], T["E2"][:], MUI, ALU.mult, r=[K_("E2"), "cst"], w=[K_("E2")])
        E.tt("pool", T["E1"][:], T["E1"][:], MLS, ALU.mult, r=[K_("E1"), "cst"], w=[K_("E1")])
        yield
        pK_, pKk = gbank()
        pK = pK_[:, 0:128]
        E.mm(pK, KnT_[:, bs], KnT_[:, bs], r=[("KnT", tp)], w=pKk)
        E.stt("dve", T["Am"][:], pK, bcol, T["E1"][:], ALU.mult, ALU.mult, r=pKk + ["beta", K_("E1")], w=[K_("Am")])
        pQ_, pQk = gbank()
        pQ = pQ_[:, 0:128]
        E.mm(pQ, KnT_[:, bs], QnT_[:, bs], r=[("KnT", tp), ("QnT", tp)], w=pQk)
        E.tt("dve", O["qkT"][:], pQ, T["E2"][:], ALU.mult, r=pQk + [K_("E2")], w=[OK_("qkT")])
        yield
        pT_, pTk = gbank()
        pT = pT_[:, 0:128]
        E.tr(pT, T["Am"][:], IDN, r=[K_("Am"), "cst"], w=pTk)
        E.copy("act", T["Bm"][:], pT, r=pTk, w=[K_("Bm")])
        E.tt("dve", T["Rm"][:], IDN, pT, ALU.subtract, r=pTk + ["cst"], w=[K_("Rm")])
        yield
        Y, YT, yk, ytk = T["Bm"], T["Am"], K_("Bm"), K_("Am")
        nxt = [("Y0", "Y0T"), ("Y1", "Y1T")]
        for lev in range(5):
            nY, nYT = nxt[lev % 2]
            last = (lev == 4)
            p2_, p2k = gbank()
            p2 = p2_[:, 0:128]
            E.mm(p2, Y[:], YT[:], r=[yk, ytk], w=p2k)
            if not last:
                p1_, p1k = gbank()
                p1 = p1_[:, 0:128]
                E.mm(p1, YT[:], Y[:], r=[yk, ytk], w=p1k)
            E.copy("dve", T[nYT][:], p2, r=p2k, w=[K_(nYT)])
            if not last:
                E.copy("act", T[nY][:], p1, r=p1k, w=[K_(nY)])
            yield
            p3_, p3k = gbank()
            p3 = p3_[:, 0:128]
            E.mm(p3, T[nYT][:], T["Rm"][:], r=[K_(nYT), K_("Rm")], w=p3k)
            E.tt("dve", T["Rm"][:], T["Rm"][:], p3, ALU.add, r=p3k + [K_("Rm")], w=[K_("Rm")])
            Y, YT, yk, ytk = T[nY], T[nYT], K_(nY), K_(nYT)
            yield
        E.act(sc3_[:, 0:1], cumc_[:, 0:1], AF.Exp, r=[K_("cumc")], w=[K_("sc3a")])
        E.tt("dve", sc3_[:, 1:2], sc3_[:, 0:1], bcol, ALU.mult, r=[K_("sc3a"), "beta"], w=[K_("sc3b")])
        E.act(sc3_[:, 2:3], cumc_[:, 0:1], AF.Exp, bias=cumc_[:, 1:2], scale=-1.0, r=[K_("cumc")], w=[K_("sc3c")])
        E.ts("pool", T["kb"][:], Kn[:, b, :], sc3_[:, 1:2], ALU.mult, r=[("Kn", b), K_("sc3b")], w=[K_("kb")])
        E.ts("pool", O["kdec"][:], Kn[:, b, :], sc3_[:, 2:3], ALU.mult, r=[("Kn", b), K_("sc3c")], w=[OK_("kdec")])
        E.ts("dve", bv_[:], VBA[:, b, 0:64], bcol, ALU.mult, r=[("VBA", b), "beta"], w=[K_("bv")])
        yield
        pU, pUk = gbank()
        E.mm(pU[:, 0:64], T["Rm"][:], bv_[:], r=[K_("Rm"), K_("bv")], w=pUk)
        E.copy("act", Ub_[:], pU[:, 0:64], r=pUk, w=[OK_("Ub")])
        pW, pWk = gbank()
        E.mm(pW[:, 0:128], T["kb"][:], T["Rm"][:], r=[K_("kb"), K_("Rm")], w=pWk)
        E.copy("act", O["WT"][:], pW[:, 0:128], r=pWk, w=[OK_("WT")])
        E.tt("pool", O["QdT"][:], QnT_[:, bs], T["ecB"][:], ALU.mult, r=[("QnT", tp), K_("ecB")], w=[OK_("QdT")])
        yield

    def dn_seq(t):
        tp = t % 2
        osb_ = osb2[tp]
        for b in range(NBT):
            O = outs[tp][b]
            OK_ = lambda n: (n, tp, b)
            Ub_, cdt_ = Ub4[tp][b], cdt4[tp][b]
            for c in range(2):
                cs_ = slice(c * 64, (c + 1) * 64)
                p4, p4k = gbank()
                E.mm(p4[cs_, 0:64], O["WT"][:, cs_], Sst[:], r=[OK_("WT"), "S"], w=p4k)
                E.tt("dve", ut[cs_, :], Ub_[cs_, :], p4[cs_, 0:64], ALU.subtract, r=p4k + [OK_("Ub")], w=["ut"])
                yield
                p5, p5k = gbank()
                E.mm(p5[cs_, 0:64], O["QdT"][:, cs_], Sst[:], start=True, stop=False, r=[OK_("QdT"), "S"], w=p5k)
                E.mm(p5[cs_, 0:64], O["qkT"][cs_, cs_], ut[cs_, :], start=False, stop=True, r=[OK_("qkT"), "ut"], w=p5k)
                p6, p6k = gbank()
                E.mm(p6[:, 0:64], O["kdec"][cs_, :], ut[cs_, :], r=[OK_("kdec"), "ut"], w=p6k)
                E.copy("act", osb_[cs_, b, :], p5[cs_, 0:64], r=p5k, w=[("osb", tp)])
                E.stt("dve", Sst[:], Sst[:], cdt_[:, c:c + 1], p6[:, 0:64], ALU.mult, ALU.add, r=p6k + ["S", OK_("cdt")], w=["S"])
                yield
        E.dma("sp", ("osb", tp), odn_d.rearrange("(b p) d -> p b d", p=128)[:, t * NBT:(t + 1) * NBT, :], osb_[:], r=[("osb", tp)], w=[("odn_out", tp)])
        yield

    def attention(s):
        NKB = 4 * (s + 1)
        accs = []
        for a in range(4):
            m_, sub_ = a // 2, a % 2
            accs.append((PS.banks[2 + sub_][:, m_ * 129:m_ * 129 + 129], [("pb", 2 + sub_)]))
        first_in_bank = {2: True, 3: True}
        for kb in range(NKB):
            bank, keys = PS.bank(kb % 2)
            kt = kb // NBT
            for m in range(2):
                E.mm(bank[:, m * TL:(m + 1) * TL], KT[m][0:65, kb * 128:(kb + 1) * 128], QT[m][0:65, :],
                     r=[("KT", m, kt), ("KTones", m), ("QT", m), ("QTb", m)], w=keys)
            pt = PT[kb % 2]
            ptk = ("PT", kb % 2)
            E.act(pt[:], bank[:, 0:2 * TL], AF.Exp, r=keys, w=[ptk])
            j = kb - (NKB - 4)
            if j >= 0:
                for m in range(2):
                    if j < 2:
                        E.stt("dve", pt[:, m * TL:(m + 1) * TL], dmask[:, j, :], HALF, pt[:, m * TL:(m + 1) * TL], ALU.max, ALU.mult,
                              r=[ptk, "dmask", "hc2d"], w=[ptk])
                    else:
                        E.stt("dve", pt[:, m * TL:(m + 1) * TL], dmask[:, j - 2, :], HALF, pt[:, m * TL:(m + 1) * TL], ALU.mult, ALU.mult,
                              r=[ptk, "dmask", "hc2d"], w=[ptk])
            for sub in range(2):
                last = 4 * s + 2 + sub
                if kb > last:
                    continue
                for m in range(2):
                    acc, ak = accs[m * 2 + sub]
                    st_ = first_in_bank[2 + sub]
                    first_in_bank[2 + sub] = False
                    P.op("pe", lambda e, acc=acc, lh=pt[:, m * TL + sub * 128:m * TL + (sub + 1) * 128], rh=VA[:, kb, :], st_=st_, sp_=(kb == last):
                         e.matmul(acc, lhsT=lh, rhs=rh, start=st_, stop=sp_, skip_group_check=True),
                         r=[ptk, ("VA", kb), "VAones"], w=ak, skip_same=True)
            yield
        for sub in range(2):
            a0, a0k = accs[sub]
            a1, a1k = accs[2 + sub]
            P.op("dve", lambda e, a0=a0, sub=sub: e.reciprocal(out=rec[:, sub * 2:sub * 2 + 1], in_=a0[:, 128:129]), r=a0k, w=[("rec", sub, 0)])
            P.op("dve", lambda e, a1=a1, sub=sub: e.reciprocal(out=rec[:, sub * 2 + 1:sub * 2 + 2], in_=a1[:, 128:129]), r=a1k, w=[("rec", sub, 1)])
            E.tt("dve", rec[:, sub * 2 + 1:sub * 2 + 2], rec[:, sub * 2 + 1:sub * 2 + 2], NLAM, ALU.mult, r=[("rec", sub, 1), "hc2c"], w=[("rec", sub, 1)])
            E.ts("dve", o1t[:], a0[:, 0:128], rec[:, sub * 2:sub * 2 + 1], ALU.mult, r=a0k + [("rec", sub, 0)], w=["o1t"])
            E.stt("dve", odasb[:, sub, :], a1[:, 0:128], rec[:, sub * 2 + 1:sub * 2 + 2], o1t[:], ALU.mult, ALU.add,
                  r=a1k + [("rec", sub, 1), "o1t"], w=["odasb"])
            yield
        E.dma("sp", "odasb", oda_d.rearrange("(b p) d -> p b d", p=128)[:, s * 2:(s + 1) * 2, :], odasb[:], r=["odasb"], w=["oda_out"])
        yield

    def chain(*gens):
        for g in gens:
            yield from g

    def rr(gens):
        gens = [g for g in gens if g is not None]
        while gens:
            for g in list(gens):
                try:
                    next(g)
                except StopIteration:
                    gens.remove(g)

    for i in range(NT + 2):
        gens = []
        if i < NT:
            gens.append(chain(front(i), q_slot(i // 2)) if i % 2 == 1 else front(i))
        if 0 <= i - 1 < NT:
            t = i - 1
            def pre_all(t=t):
                yield from dn_head(t)
                g0, g1 = dn_pre(t, 0), dn_pre(t, 1)
                alive = [g0, g1]
                while alive:
                    for g in list(alive):
                        try:
                            next(g)
                        except StopIteration:
                            alive.remove(g)
                    yield
            gens.append(pre_all())
        if 0 <= i - 2 < NT:
            gens.append(dn_seq(i - 2))
        if i >= 2 and i % 2 == 0 and (i - 2) // 2 < NS:
            gens.append(attention((i - 2) // 2))
        rr(gens)
    P.final_wait("sp")
    P.run()
    P.close()
    return nc


def host_inputs_a(inp, S):
    x = inp["x"][0, :S]
    SQ = S // 2
    NT = S // TL
    xT = np.ascontiguousarray(x.T)
    c = inp["c"][0]
    c_col = np.ascontiguousarray(c.reshape(8, 128).T)
    w_ada_a = np.ascontiguousarray(inp["w_ada"][0][:, 0:2048])
    b_ada_col = np.ascontiguousarray(inp["b_ada"][0][0:2048].reshape(16, 128).T)
    w_in = inp["w_in"][0]
    conv_w = inp["conv_w"][0]
    cst, _ = _consts_f32()
    pos = np.ascontiguousarray(inp["positions"][:, :S]).astype(np.int32)
    p = np.arange(128)[:, None]
    f = np.arange(TL)[None, :]
    dm = np.concatenate([((128 * j + p) <= f).astype(np.float32) for j in range(2)], axis=1)
    lam4 = np.concatenate([inp["lambda_q1"][0], inp["lambda_k1"][0], inp["lambda_q2"][0], inp["lambda_k2"][0]])[None, :]
    lam4 = np.ascontiguousarray(np.broadcast_to(lam4, (128, 256))).astype(np.float32)
    lam_init = 0.8 - 0.6 * np.exp(-0.3 * 0)
    maps = []
    for i in range(NCORES):
        h, half = i % 4, i // 4
        o_q, o_k, o_v, o_z, o_b, o_a, o_aq, o_ak, o_av = 0, 512, 1024, 1536, 2048, 2052, 2056, 2568, 3080
        cols = np.concatenate([
            o_q + h * 128 + np.arange(128), o_k + h * 128 + np.arange(128),
            o_v + h * 128 + half * 64 + np.arange(64), [o_b + h], [o_a + h],
            o_aq + h * 128 + np.arange(128), o_ak + h * 128 + np.arange(128), o_av + h * 128 + np.arange(128)])
        w_a = np.ascontiguousarray(w_in[:, cols])
        conv_col = np.zeros((128, 3, 4), np.float32)
        conv_col[:, 0, :] = conv_w[:, h * 128 + np.arange(128)].T
        conv_col[:, 1, :] = conv_w[:, 512 + h * 128 + np.arange(128)].T
        conv_col[0:64, 2, :] = conv_w[:, 1024 + h * 128 + half * 64 + np.arange(64)].T
        conv_col[64:66, 2, 3] = 1.0
        headc = np.zeros((128, 4), np.float32)
        headc[:, 0] = inp["dn_a_log"][0, h]
        headc[:, 1] = inp["dn_dt_bias"][0, h]
        headc[:, 2] = float(half)
        headc[:, 3] = lam_init
        own_tiles = [2 * s + half for s in range(NT // 2)]
        own_idx = np.concatenate([np.arange(t * TL, (t + 1) * TL) for t in own_tiles])
        maps.append({
            "xT": xT, "xT_own": np.ascontiguousarray(xT[:, own_idx]), "c_col": c_col, "w_ada_a": w_ada_a,
            "b_ada_col": b_ada_col, "w_a": w_a, "conv_col": conv_col.reshape(128, 12), "headc": headc, "lam4": lam4,
            "pos": pos, "pos_own": np.ascontiguousarray(pos[:, own_idx]), "dmask": dm, "consts": cst,
        })
    return maps


def gather_a(results, S):
    NT = S // TL
    o_dn = np.zeros((S, 512), np.float32)
    o_da = np.zeros((S, 512), np.float32)
    for i in range(NCORES):
        h, half = i % 4, i // 4
        o_dn[:, h * 128 + half * 64:h * 128 + half * 64 + 64] = results[i]["o_dn"]
        own_tiles = [2 * s + half for s in range(NT // 2)]
        own_idx = np.concatenate([np.arange(t * TL, (t + 1) * TL) for t in own_tiles])
        o_da[own_idx, h * 128:(h + 1) * 128] = results[i]["o_da"]
    return o_dn, o_da


ALPHA = 2.0 ** 0.25
LAM_INIT0 = 0.8 - 0.6 * 1.0
NE = 32


def build_phase_b(NTOK, GT=None):
    TPC = NTOK // 128
    GT = GT or min(8, TPC)
    NG = TPC // GT
    nc = bass.Bass("TRN2", target_bir_lowering=False)
    dr = lambda name, shape, dt, kind="ExternalInput": nc.dram_tensor(name, list(shape), dt, kind=kind).ap()
    x_d = dr("x_tok", [NTOK, D], F32)
    odn_d = dr("odn", [NTOK, 512], F32)
    oda_d = dr("oda", [NTOK, 512], F32)
    ccol_d = dr("c_col", [128, 8], F32)
    wada_d = dr("w_ada", [D, 6144], F32)
    badac_d = dr("b_ada_col", [128, 48], F32)
    badar_d = dr("b_ada_row", [1, 6144], F32)
    wb_d = dr("w_b", [D, 2560], F32)
    wdn_d = dr("w_dn", [512, D], F32)
    wda_d = dr("w_da", [512, D], F32)
    wo_d = dr("w_o", [D, D], F32)
    nrm_d = dr("normw", [1, 256], F32)
    ln_d = dr("lnp", [1, 4096], F32)
    wr_d = dr("w_router", [D, NE], F32)
    br_d = dr("b_router", [1, NE], F32)
    wgu_d = dr("w_gu", [NE, 4, D, 512], F32)
    bgu_d = dr("b_gu", [NE, 2048], F32)
    wd_d = dr("w_down", [NE, D, D], F32)
    bd_d = dr("b_down", [NE, D], F32)
    cst_d = dr("consts", [128, 8 * 128], F32)
    out_d = dr("out", [NTOK, D], F32, kind="ExternalOutput")
    x1_d = dr("x1_scr", [NTOK, D], F32, kind="Internal")
    ufT_d = dr("ufT_scr", [TPC, 128, 8 * 128], BF16, kind="Internal")

    P = Prog(nc)
    E = Em(P)
    PS = PsumPool(P)
    ALLB = list(range(8))
    PAIRS = [(0, 1), (2, 3), (4, 5), (6, 7)]

    def pb1():
        b = PS.rot("one", ALLB)
        return PS.banks[b], [("pb", b)]

    def pb2():
        a, b = PS.rot("two", PAIRS)
        return (PS.banks[a], PS.banks[b]), [("pb", a), ("pb", b)]

    cst = P.sb("cst", [128, 8 * 128], F32)
    IDN = cst[:, 0:128]
    ONES = cst[:, 5 * 128:6 * 128]
    idb = P.sb("idb", [128, 128], BF16)
    onesb = P.sb("onesb", [128, 128], BF16)
    ccol = P.sb("ccol", [128, 8], F32)
    silc = P.sb("silc", [128, 8], F32)
    silcb = P.sb("silcb", [128, 8, 128], F32)
    badac = P.sb("badac", [128, 48], F32)
    mcol = P.sb("mcol", [128, 48], F32)
    gateb = P.sb("gateb", [128, 2, D], F32)
    lnb = P.sb("lnb", [128, 4, D], F32)
    nrmb = P.sb("nrmb", [128, 256], F32)
    wr = P.sb("wr", [128, 8, NE], F32)
    brb = P.sb("brb", [128, NE], F32)
    bdsb = P.sb("bdsb", [NE, D], F32)
    RW = P.sb("RW", [128, TPC, NE], F32)
    st8 = P.sb("st8", [128, 16], F32)
    AR = 40 * 1024
    arena = P.sb("arena", [128, AR], F32)
    abf = arena[:].bitcast(BF16)

    def fview(off, n):
        return arena[:, off:off + n]

    def bview(off, n):
        return abf[:, 2 * off:2 * off + n]

    o = 0
    Wb = bview(o, 8 * 2560).rearrange("p (k c) -> p k c", k=8); o += 8 * 2560 // 2
    Wdn = bview(o, 4 * D).rearrange("p (k c) -> p k c", k=4); o += 4 * D // 2
    Wda = bview(o, 4 * D).rearrange("p (k c) -> p k c", k=4); o += 4 * D // 2
    Wo = bview(o, 8 * D).rearrange("p (k c) -> p k c", k=8); o += 8 * D // 2
    F = []
    for i in range(4):
        F.append(fview(o, D)); o += D
    xT32 = fview(o, D).rearrange("p (k c) -> p k c", k=8); o += D
    ufT32 = fview(o, D).rearrange("p (k c) -> p k c", k=8); o += D
    odn_t = fview(o, 512); o += 512
    oda_t = fview(o, 512); o += 512
    sqt = fview(o, 512); o += 512
    siluz = fview(o, 512); o += 512
    uT = bview(o, D).rearrange("p (k c) -> p k c", k=8); o += D // 2
    ufTb = bview(o, D); o += D // 2
    mb = bview(o, D); o += D // 2
    mT = bview(o, D).rearrange("p (k c) -> p k c", k=8); o += D // 2
    a12 = bview(o, D); o += D // 2
    a12T = bview(o, D).rearrange("p (k c) -> p k c", k=8); o += D // 2
    wst = fview(o, 8 * 512).rearrange("p (k c) -> p k c", k=8); o += 8 * 512
    assert o <= AR, o
    o = 0
    Wgu = []
    for i in range(2):
        Wgu.append(bview(o, 4 * 8 * 512).rearrange("p (g k c) -> p g k c", g=4, k=8)); o += 4 * 8 * 512 // 2
    Wd2 = []
    for i in range(2):
        Wd2.append(bview(o, 8 * D).rearrange("p (k c) -> p k c", k=8)); o += 8 * D // 2
    ufT = bview(o, 8 * GT * 128).rearrange("p (t k c) -> p t k c", t=GT, k=8); o += 8 * GT * 128 // 2
    acc = fview(o, GT * D).rearrange("p (t c) -> p t c", t=GT); o += GT * D
    bgu2 = []
    for i in range(2):
        bgu2.append(bview(o, 1024)); o += 512
    G = []
    for i in range(4):
        G.append(fview(o, 256)); o += 256
    actv = bview(o, D); o += D // 2
    actvB = bview(o, D); o += D // 2
    actT = bview(o, D).rearrange("p (k c) -> p k c", k=8); o += D // 2
    rwT = fview(o, 128); o += 128
    H = [Wd2[0].rearrange("p k c -> p (k c)").bitcast(F32)[:, 0:D], None, None]
    assert o <= AR, o

    E.dma("sp", "cst", cst[:], cst_d[:], w=["cst"])
    E.copy("dve", idb[:], IDN, r=["cst"], w=["idb"])
    E.memset("dve", onesb[:], 1.0, w=["onesb"])
    E.dma("sp", "ccol", ccol[:], ccol_d[:], w=["ccol"])
    E.dma("sp", "badac", badac[:], badac_d[:], w=["badac"])
    E.dma("sp", "gateb0", gateb[:, 0, :], badar_d[0:1, 2048:3072].partition_broadcast(128), w=["gateb0"])
    E.dma("sp", "gateb1", gateb[:, 1, :], badar_d[0:1, 5120:6144].partition_broadcast(128), w=["gateb1"])
    E.dma("sp", "lnb", lnb[:].rearrange("p a b -> p (a b)"), ln_d[0:1, :].partition_broadcast(128), w=["lnb"])
    E.dma("sp", "nrmb", nrmb[:], nrm_d[0:1, :].partition_broadcast(128), w=["nrmb"])
    E.dma("sp", "wr", wr[:], wr_d.rearrange("(k p) e -> p k e", p=128), w=["wr"])
    E.dma("sp", "brb", brb[:], br_d[0:1, :].partition_broadcast(128), w=["brb"])
    E.dma("sp", "bdsb", bdsb[:], bd_d[:], w=["bdsb"])
    E.ts("dve", nrmb[:, 128:256], nrmb[:, 128:256], 1.0 - LAM_INIT0, ALU.mult, r=["nrmb"], w=["nrmb"])
    E.act(silc[:], ccol[:], AF.Silu, r=["ccol"], w=["silc"])
    for k in range(8):
        E.ts("dve", silcb[:, k, :], ONES, silc[:, k:k + 1], ALU.mult, r=["cst", "silc"], w=[("silcb", k)])
    SCB = [("silcb", k) for k in range(8)]
    for pc in range(12):
        mod, hf = pc // 2, pc % 2
        E.dma("sp", "wst", wst[:], wada_d.rearrange("(k p) c -> p k c", p=128)[:, :, pc * 512:(pc + 1) * 512], w=["wst"])
        if mod in (2, 5):
            gi = 0 if mod == 2 else 1
            bank, keys = pb1()
            for k in range(8):
                E.mm(bank[:, :], silcb[:, k, :], wst[:, k, :], start=(k == 0), stop=(k == 7), r=["wst"] + SCB, w=keys)
            E.tt("dve", gateb[:, gi, hf * 512:(hf + 1) * 512], gateb[:, gi, hf * 512:(hf + 1) * 512], bank[:, :], ALU.add,
                 r=keys + ["gateb%d" % gi], w=["gateb%d" % gi])
        else:
            bank, keys = pb1()
            for jj in range(4):
                for k in range(8):
                    E.mm(bank[:, jj:jj + 1], wst[:, k, jj * 128:(jj + 1) * 128], silc[:, k:k + 1], start=(k == 0), stop=(k == 7),
                         r=["wst", "silc"], w=keys)
            j0 = mod * 8 + hf * 4
            E.tt("dve", mcol[:, j0:j0 + 4], bank[:, 0:4], badac[:, j0:j0 + 4], ALU.add, r=keys + ["badac"], w=[("mcol", pc)])
    MC = [("mcol", pc) for pc in range(12)]
    E.ts("dve", mcol[:, 8:16], mcol[:, 8:16], 1.0, ALU.add, r=MC, w=MC)
    E.ts("dve", mcol[:, 32:40], mcol[:, 32:40], 1.0, ALU.add, r=MC, w=MC)
    E.dma("pool", "Wb", Wb, wb_d.rearrange("(k p) c -> p k c", p=128), r=["wst"], w=["Wb"])
    E.dma("pool", "Wdn", Wdn, wdn_d.rearrange("(k p) c -> p k c", p=128), w=["Wdn"])
    E.dma("pool", "Wda", Wda, wda_d.rearrange("(k p) c -> p k c", p=128), w=["Wda"])
    E.dma("pool", "Wo", Wo, wo_d.rearrange("(k p) c -> p k c", p=128), w=["Wo"])

    def transpose_mod(src, src_keys, dst_bf, dst_bf_key, sh0, sc0, dst32=None, dst32_key=None):
        (b0, b1), keys = pb2()
        for k in range(8):
            bk = b0 if k < 4 else b1
            E.tr(bk[:, (k % 4) * 128:(k % 4 + 1) * 128], src[:, k * 128:(k + 1) * 128], IDN, r=src_keys + ["cst"], w=keys)
        for k in range(8):
            bk = b0 if k < 4 else b1
            pin = bk[:, (k % 4) * 128:(k % 4 + 1) * 128]
            E.act(dst_bf[:, k, :], pin, AF.Identity, bias=mcol[:, sh0 + k:sh0 + k + 1], scale=mcol[:, sc0 + k:sc0 + k + 1],
                  r=keys + MC, w=[dst_bf_key])
            if dst32 is not None:
                E.ts("dve", dst32[:, k, :], pin, mcol[:, sc0 + k:sc0 + k + 1], ALU.mult, mcol[:, sh0 + k:sh0 + k + 1], ALU.add,
                     r=keys + MC, w=[dst32_key])

    def transpose_bf(src_bf, src_keys, dst, dst_key, nk):
        bank, keys = pb1()
        pb = bank[:].bitcast(BF16)
        for k in range(nk):
            E.tr(pb[:, k * 128:(k + 1) * 128], src_bf[:, k * 128:(k + 1) * 128], idb[:], r=src_keys + ["idb"], w=keys)
        E.copy("act", dst[:, 0:nk, :].rearrange("p k c -> p (k c)"), pb[:, 0:nk * 128], r=keys, w=[dst_key])

    def rmsnorm_heads(src, src_key, nw, out_bf, out_key, mul=None, mul_key=None):
        E.tt("dve", sqt, src, src, ALU.mult, r=[src_key], w=["sqt"])
        P.op("dve", lambda e: e.reduce_sum(out=st8[:, 0:4], in_=sqt.rearrange("p (h c) -> p h c", h=4), axis=AX.X), r=["sqt"], w=["st8a"])
        E.act(st8[:, 4:8], st8[:, 0:4], AF.Sqrt, bias=EPS_N, scale=1.0 / 128.0, r=["st8a"], w=["st8b"])
        P.op("dve", lambda e: e.reciprocal(out=st8[:, 4:8], in_=st8[:, 4:8]), r=["st8b"], w=["st8b"])
        for h in range(4):
            hs = slice(h * 128, (h + 1) * 128)
            if mul is None:
                E.stt("dve", out_bf[:, hs], src[:, hs], st8[:, 4 + h:5 + h], nw, ALU.mult, ALU.mult, r=[src_key, "st8b", "nrmb"], w=[out_key])
            else:
                E.stt("dve", sqt[:, hs], src[:, hs], st8[:, 4 + h:5 + h], nw, ALU.mult, ALU.mult, r=[src_key, "st8b", "nrmb", "sqt"], w=["sqt"])
        if mul is not None:
            E.tt("dve", out_bf, sqt, mul, ALU.mult, r=["sqt", mul_key], w=[out_key])

    def layer_norm(src, src_key, gi, dst, dst_key, tmp, tmp_key):
        P.op("dve", lambda e: e.reduce_sum(out=st8[:, 8:9], in_=src, axis=AX.X), r=[src_key], w=["st8c"])
        E.ts("dve", st8[:, 9:10], st8[:, 8:9], -1.0 / D, ALU.mult, r=["st8c"], w=["st8d"])
        E.ts("dve", tmp, src, st8[:, 9:10], ALU.add, r=[src_key, "st8d"], w=[tmp_key])
        E.act(dst, tmp, AF.Square, r=[tmp_key], w=[dst_key], accum_out=st8[:, 10:11])
        E.act(st8[:, 11:12], st8[:, 10:11], AF.Sqrt, bias=EPS_N, scale=1.0 / D, r=[dst_key], w=["st8e"])
        P.op("dve", lambda e: e.reciprocal(out=st8[:, 11:12], in_=st8[:, 11:12]), r=["st8e"], w=["st8e"])
        E.stt("dve", dst, tmp, st8[:, 11:12], lnb[:, gi, :], ALU.mult, ALU.mult, r=[tmp_key, "st8e", "lnb"], w=[dst_key])
        E.tt("dve", dst, dst, lnb[:, gi + 1, :], ALU.add, r=[dst_key, "lnb"], w=[dst_key])

    def stage1(t):
        rows = slice(t * 128, (t + 1) * 128)
        xt, sg, m1, r_ = F
        E.dma("sp", "xt", xt, x_d[rows, :], w=["xt"])
        E.dma("sp", "odn_t", odn_t, odn_d[rows, :], w=["odn_t"])
        E.dma("sp", "oda_t", oda_t, oda_d[rows, :], w=["oda_t"])
        transpose_mod(xt, ["xt"], uT, "uT", 0, 8)
        bank, keys = pb1()
        for k in range(8):
            E.mm(bank[:, :], uT[:, k, :], Wb[:, k, 0:512], start=(k == 0), stop=(k == 7), r=["uT", "Wb"], w=keys)
        E.act(siluz, bank[:, :], AF.Silu, r=keys, w=["siluz"])
        rmsnorm_heads(odn_t, "odn_t", nrmb[:, 0:128], a12[:, 0:512], "a1", mul=siluz, mul_key="siluz")
        rmsnorm_heads(oda_t, "oda_t", nrmb[:, 128:256], a12[:, 512:1024], "a2")
        transpose_bf(a12, ["a1", "a2"], a12T, "a12T", 8)
        for br in range(2):
            (g0, g1), gk = pb2()
            for hf, bk in enumerate((g0, g1)):
                c0 = 512 + br * 1024 + hf * 512
                for k in range(8):
                    E.mm(bk[:, :], uT[:, k, :], Wb[:, k, c0:c0 + 512], start=(k == 0), stop=(k == 7), r=["uT", "Wb"], w=gk)
            for hf, bk in enumerate((g0, g1)):
                E.act(sg[:, hf * 512:(hf + 1) * 512], bk[:, :], AF.Sigmoid, r=gk, w=["sg"])
            (y0, y1), yk = pb2()
            Wp, wkey = (Wdn, "Wdn") if br == 0 else (Wda, "Wda")
            for hf, bk in enumerate((y0, y1)):
                for kc in range(4):
                    E.mm(bk[:, :], a12T[:, br * 4 + kc, :], Wp[:, kc, hf * 512:(hf + 1) * 512], start=(kc == 0), stop=(kc == 3),
                         r=["a12T", wkey], w=yk)
            for hf, bk in enumerate((y0, y1)):
                hs = slice(hf * 512, (hf + 1) * 512)
                if br == 0:
                    E.tt("dve", m1[:, hs], sg[:, hs], bk[:, :], ALU.mult, r=yk + ["sg"], w=["m1"])
                else:
                    E.tt("dve", sg[:, hs], sg[:, hs], bk[:, :], ALU.mult, r=yk + ["sg"], w=["sg"])
        E.tt("dve", mb, m1, sg, ALU.add, r=["m1", "sg"], w=["mb"])
        transpose_bf(mb, ["mb"], mT, "mT", 8)
        (o0, o1), ok = pb2()
        for hf, bk in enumerate((o0, o1)):
            for k in range(8):
                E.mm(bk[:, :], mT[:, k, :], Wo[:, k, hf * 512:(hf + 1) * 512], start=(k == 0), stop=(k == 7), r=["mT", "Wo"], w=ok)
        for hf, bk in enumerate((o0, o1)):
            hs = slice(hf * 512, (hf + 1) * 512)
            E.tt("dve", r_[:, hs], bk[:, :], gateb[:, 0, hs], ALU.mult, r=ok + ["gateb0"], w=["r_"])
        E.stt("dve", r_, xt, ALPHA, r_, ALU.mult, ALU.add, r=["xt", "r_"], w=["r_"])
        layer_norm(r_, "r_", 0, m1, "m1", sg, "sg")
        E.dma("sp", "x1out", x1_d[rows, :], m1, r=["m1"], w=["x1d"])
        transpose_mod(m1, ["m1"], ufTb.rearrange("p (k c) -> p k c", k=8), "ufTb", 24, 32, dst32=ufT32, dst32_key="ufT32")
        E.dma("sp", "ufTout", ufT_d[t], ufTb, r=["ufTb"], w=["ufTd"])
        bank, keys = pb1()
        for k in range(8):
            E.mm(bank[:, 0:NE], ufT32[:, k, :], wr[:, k, :], start=(k == 0), stop=(k == 7), r=["ufT32", "wr"], w=keys)
        lg = sqt[:, 0:NE]
        E.tt("dve", lg, bank[:, 0:NE], brb[:], ALU.add, r=keys + ["brb", "sqt"], w=["sqt"])
        P.op("dve", lambda e: e.max(out=st8[:, 0:8], in_=lg), r=["sqt"], w=["st8a"])
        E.ts("dve", sqt[:, 32:64], lg, st8[:, 3:4], ALU.is_ge, r=["sqt", "st8a"], w=["sqt"])
        E.ts("dve", st8[:, 12:13], st8[:, 0:1], -1.0, ALU.mult, r=["st8a"], w=["st8f"])
        E.act(sqt[:, 64:96], lg, AF.Exp, bias=st8[:, 12:13], r=["sqt", "st8f"], w=["sqt"])
        E.tt("dve", sqt[:, 64:96], sqt[:, 64:96], sqt[:, 32:64], ALU.mult, r=["sqt"], w=["sqt"])
        P.op("dve", lambda e: e.reduce_sum(out=st8[:, 13:14], in_=sqt[:, 64:96], axis=AX.X), r=["sqt"], w=["st8g"])
        P.op("dve", lambda e: e.reciprocal(out=st8[:, 13:14], in_=st8[:, 13:14]), r=["st8g"], w=["st8g"])
        E.ts("dve", RW[:, t, :], sqt[:, 64:96], st8[:, 13:14], ALU.mult, r=["sqt", "st8g"], w=[("RW", t)])

    def barrier():
        toks = {}
        for k, st in P.res.items():
            for tk in ([st[0]] if st[0] is not None else []) + st[1]:
                key = id(tk[0])
                if key not in toks or toks[key][1] < tk[1]:
                    toks[key] = tk
        for eng in ENGS:
            P.ops[eng].append(([(s, v) for (s, v, e) in toks.values()], None, None, None))
            for (s, v, e) in toks.values():
                P.known[eng][id(s)] = max(P.known[eng].get(id(s), -1), v)

    def load_expert(e, first):
        wb_ = Wgu[e % 2]
        for cg in range(4):
            E.dma("pool", ("Wgu", e % 2, cg), wb_[:, cg, :, :], wgu_d[e, cg].rearrange("(k p) c -> p k c", p=128), w=[("Wgu", e % 2, cg)])

    def moe_group(g):
        for tt_ in range(GT):
            E.dma("sp", ("ufTin", tt_), ufT[:, tt_, :, :].rearrange("p k c -> p (k c)"), ufT_d[g * GT + tt_], r=["ufTd"], w=[("ufT", tt_)])
        for tt_ in range(GT):
            t = g * GT + tt_
            bank, keys = pb1()
            E.tr(bank[0:NE, 0:128], RW[:, t, :], IDN, r=[("RW", t), "cst"], w=keys)
            E.copy("act", rwT[0:NE, :], bank[0:NE, 0:128], r=keys, w=["rwT"])
            (a0, a1), ak = pb2()
            for hf, bk in enumerate((a0, a1)):
                E.mm(bk[:, :], rwT[0:NE, :], bdsb[:, hf * 512:(hf + 1) * 512], r=["rwT", "bdsb"], w=ak)
            for hf, bk in enumerate((a0, a1)):
                E.copy("act", acc[:, tt_, hf * 512:(hf + 1) * 512], bk[:, :], r=ak, w=[("acc", tt_)])
        actv2 = [actv, actvB]

        def gate_up(e, tt_, u):
            t = g * GT + tt_
            wb_ = Wgu[e % 2]
            av = actv2[u % 2]
            for cg in range(4):
                bank, keys = pb1()
                for k in range(8):
                    E.mm(bank[:, :], ufT[:, tt_, k, :], wb_[:, cg, k, :], start=(k == 0), stop=False,
                         r=[("ufT", tt_), ("Wgu", e % 2, cg)], w=keys)
                E.mm(bank[:, :], onesb[32 * (cg % 2):32 * (cg % 2) + 1, :], bgu2[e % 2][32 * (cg % 2):32 * (cg % 2) + 1, 512 * (cg // 2):512 * (cg // 2) + 512], start=False, stop=True, r=["onesb", ("bgu", e % 2, cg)], w=keys)
                gl, sgm, lnn, t3 = G
                E.ts("dve", gl, bank[:, 0:256], 7.0, ALU.min, r=keys, w=["gl"])
                E.ts("dve", lnn, bank[:, 256:512], -7.0, ALU.max, 7.0, ALU.min, r=keys, w=["lnn"])
                E.act(sgm, gl, AF.Sigmoid, scale=1.702, r=["gl"], w=["sgm"])
                E.stt("dve", t3, lnn, 1.0, gl, ALU.add, ALU.mult, r=["lnn", "gl"], w=["t3"])
                E.stt("dve", av[:, cg * 256:(cg + 1) * 256], t3, RW[:, t, e:e + 1], sgm, ALU.mult, ALU.mult,
                      r=["t3", "sgm", ("RW", t)], w=[("actv", u % 2, cg)])

        def tr_down(e, tt_, u):
            av = actv2[u % 2]
            transpose_bf(av, [("actv", u % 2, cg) for cg in range(4)], actT, "actT", 8)
            (d0, d1), dk = pb2()
            for hf, bk in enumerate((d0, d1)):
                for kc in range(8):
                    E.mm(bk[:, :], actT[:, kc, :], Wd2[e % 2][:, kc, hf * 512:(hf + 1) * 512], start=(kc == 0), stop=(kc == 7),
                         r=["actT", ("Wd", e % 2)], w=dk)
            for hf, bk in enumerate((d0, d1)):
                hs = slice(hf * 512, (hf + 1) * 512)
                E.tt("dve", acc[:, tt_, hs], acc[:, tt_, hs], bk[:, :], ALU.add, r=dk + [("acc", tt_)], w=[("acc", tt_)])

        def load_all(e):
            load_expert(e, False)
            E.dma("pool", ("Wd", e % 2), Wd2[e % 2], wd_d[e].rearrange("(k p) c -> p k c", p=128), w=[("Wd", e % 2)])
            for cg in range(4):
                E.dma("pool", ("bgu", e % 2, cg), bgu2[e % 2][32 * (cg % 2):32 * (cg % 2) + 1, 512 * (cg // 2):512 * (cg // 2) + 512], bgu_d[e:e + 1, cg * 512:(cg + 1) * 512], w=[("bgu", e % 2, cg)])

        units = [(e, tt_) for e in range(NE) for tt_ in range(GT)]
        load_all(0)
        load_all(1)
        gate_up(units[0][0], units[0][1], 0)
        for u, (e, tt_) in enumerate(units):
            if u + 1 < len(units):
                e2, t2 = units[u + 1]
                gate_up(e2, t2, u + 1)
            tr_down(e, tt_, u)
            if tt_ == GT - 1 and e + 2 < NE:
                load_all(e + 2)
        for tt_ in range(GT):
            t = g * GT + tt_
            rows = slice(t * 128, (t + 1) * 128)
            x1t = H[0]
            xk_ = ("Wd", 0)
            r2 = acc[:, tt_, :]
            tmp = Wgu[0][:, 0, :, :].rearrange("p k c -> p (k c)").bitcast(F32)[:, 0:D]
            E.dma("sp", "x1in", x1t, x1_d[rows, :], r=["x1d"], w=[xk_])
            E.tt("dve", r2, acc[:, tt_, :], gateb[:, 1, :], ALU.mult, r=[("acc", tt_), "gateb1"], w=[("acc", tt_)])
            E.stt("dve", r2, x1t, ALPHA, r2, ALU.mult, ALU.add, r=[xk_, ("acc", tt_)], w=[("acc", tt_)])
            layer_norm(r2, ("acc", tt_), 2, x1t, xk_, tmp, ("Wgu", 0, 0))
            E.dma("sp", "outd", out_d[rows, :], x1t, r=[xk_], w=["outd"])

    for t in range(TPC):
        stage1(t)
    barrier()
    for g in range(NG):
        moe_group(g)
    P.final_wait("sp")
    P.run()
    P.close()
    return nc


def host_inputs_b(inp, S, o_dn, o_da):
    NTOK = S // NCORES
    x = inp["x"][0, :S]
    c = inp["c"][0]
    cst, _ = _consts_f32()
    wg = inp["w_gate_up"][0]
    idx = np.concatenate([np.concatenate([2 * (256 * cg + np.arange(256)), 2 * (256 * cg + np.arange(256)) + 1]) for cg in range(4)])
    w_gu = np.ascontiguousarray(wg[:, :, idx].reshape(NE, D, 4, 512).transpose(0, 2, 1, 3))
    b_gu = np.ascontiguousarray(inp["b_gate_up"][0][:, idx])
    w_in = inp["w_in"][0]
    w_b = np.ascontiguousarray(np.concatenate([w_in[:, 1536:2048], w_in[:, 3592:4616], w_in[:, 4616:5640]], axis=1))
    shared = {
        "c_col": np.ascontiguousarray(c.reshape(8, 128).T), "w_ada": np.ascontiguousarray(inp["w_ada"][0]),
        "b_ada_col": np.ascontiguousarray(inp["b_ada"][0].reshape(48, 128).T), "b_ada_row": np.ascontiguousarray(inp["b_ada"]),
        "w_b": w_b, "w_dn": np.ascontiguousarray(inp["w_dn_proj"][0]), "w_da": np.ascontiguousarray(inp["w_da_proj"][0]),
        "w_o": np.ascontiguousarray(inp["w_o"][0]),
        "normw": np.ascontiguousarray(np.concatenate([inp["dn_norm_w"][0], inp["da_norm_w"][0]])[None, :]),
        "lnp": np.ascontiguousarray(np.concatenate([inp["ln1_g"][0], inp["ln1_b"][0], inp["ln2_g"][0], inp["ln2_b"][0]])[None, :]),
        "w_router": np.ascontiguousarray(inp["w_router"][0]), "b_router": np.ascontiguousarray(inp["b_router"]),
        "w_gu": w_gu, "b_gu": b_gu, "w_down": np.ascontiguousarray(inp["w_down"][0]), "b_down": np.ascontiguousarray(inp["b_down"][0]),
        "consts": cst,
    }
    maps = []
    for i in range(NCORES):
        rows = slice(i * NTOK, (i + 1) * NTOK)
        m = dict(shared)
        m["x_tok"] = np.ascontiguousarray(x[rows])
        m["odn"] = np.ascontiguousarray(o_dn[rows])
        m["oda"] = np.ascontiguousarray(o_da[rows])
        maps.append(m)
    return maps


def kernel(**inp):
    inp = {k: np.asarray(v) for k, v in inp.items()}
    S = inp["x"].shape[1]
    nca = build_phase_a(S)
    ra = run_bass_kernel_spmd(nca, host_inputs_a(inp, S), core_ids=list(range(NCORES)))
    o_dn, o_da = gather_a(ra.results, S)
    ncb = build_phase_b(S // NCORES)
    rb = run_bass_kernel_spmd(ncb, host_inputs_b(inp, S, o_dn, o_da), core_ids=list(range(NCORES)))
    out = np.concatenate([rb.results[i]["out"] for i in range(NCORES)], axis=0)
    return out[None].astype(np.float32)
```
